# Optimizing a Trainium2 kernel written in Bass

```python
import jax, jax.numpy as jnp
from jax import lax
import numpy as np

D_MODEL = 1024
BATCH = 4
SEQ = 8192
DEPTH = 1

PLE_DIM = 256
GLA_HEADS = 4
GLA_DK = 64
GLA_DV = 128
GLA_KEY = GLA_HEADS * GLA_DK
GLA_VAL = GLA_HEADS * GLA_DV
GLA_RANK = 16
GLA_TAU = 16.0
GLA_CHUNK = 64
CONV_DIM = 512
CONV_GROUPS = 8
CONV_K = 3
D_MIX = GLA_VAL + CONV_DIM
IN_SPLIT_SIZES = (GLA_KEY, GLA_KEY, GLA_VAL, GLA_VAL, GLA_RANK, CONV_DIM, CONV_DIM, CONV_DIM)
D_IN = sum(IN_SPLIT_SIZES)
D_FF = 2816
FFN_K = 3
EPS = 1e-6

kernel_name = "hybrid_gla_shortconv_convffn_ple"


def rmsnorm(x, g):
    xf = x.astype(jnp.float32)
    y = xf * lax.rsqrt(jnp.mean(xf * xf, axis=-1, keepdims=True) + EPS)
    return (y * g.astype(jnp.float32)).astype(x.dtype)


def causal_dwconv(u, w):
    K = w.shape[0]
    S = u.shape[1]
    up = jnp.pad(u, ((0, 0), (K - 1, 0), (0, 0)))
    out = w[K - 1] * u
    for k in range(K - 1):
        out = out + w[k] * up[:, k:k + S]
    return out


def gla_chunked(q, k, v, log_a):
    Bn, S, H, dk = q.shape
    dv = v.shape[-1]
    N = S // GLA_CHUNK

    def to_chunks(t):
        return t.astype(jnp.float32).reshape(Bn, N, GLA_CHUNK, H, t.shape[-1]).transpose(0, 3, 1, 2, 4)

    qc = to_chunks(q) * (dk ** -0.5)
    kc = to_chunks(k)
    vc = to_chunks(v)
    gc = to_chunks(log_a)
    b = jnp.cumsum(gc, axis=-2)
    b_last = b[..., -1:, :]
    q_d = qc * jnp.exp(b)
    k_d = kc * jnp.exp(-b)
    k_end = kc * jnp.exp(b_last - b)

    causal = jnp.tril(jnp.ones((GLA_CHUNK, GLA_CHUNK), dtype=bool))
    scores = jnp.einsum('bhnid,bhnjd->bhnij', q_d, k_d)
    scores = jnp.where(causal, scores, 0.0)
    o_intra = jnp.einsum('bhnij,bhnjv->bhniv', scores, vc)

    U = jnp.einsum('bhnjd,bhnjv->bhndv', k_end, vc)
    decay = jnp.exp(b_last[..., 0, :])

    def step(state, inp):
        dec_n, u_n = inp
        new = dec_n[..., None] * state + u_n
        return new, state

    s0 = jnp.zeros((Bn, H, dk, dv), jnp.float32)
    _, s_prev = lax.scan(step, s0, (jnp.moveaxis(decay, 2, 0), jnp.moveaxis(U, 2, 0)))
    s_prev = jnp.moveaxis(s_prev, 0, 2)
    o_inter = jnp.einsum('bhnid,bhndv->bhniv', q_d, s_prev)
    o = o_intra + o_inter
    return o.transpose(0, 2, 3, 1, 4).reshape(Bn, S, H, dv)


def setup_inputs(seed: int = 0) -> dict:
    key = jax.random.key(seed)
    ks = jax.random.split(key, 20)
    f32 = jnp.float32

    def w(k, shape, fan_in):
        return jax.random.normal(k, shape, f32) * (fan_in ** -0.5)

    def gain(k, shape):
        return 1.0 + 0.02 * jax.random.normal(k, shape, f32)

    return {
        "x": jax.random.normal(ks[0], (BATCH, SEQ, D_MODEL), f32),
        "p": jax.random.normal(ks[1], (DEPTH, BATCH, SEQ, PLE_DIM), f32),
        "attn_norm": gain(ks[2], (DEPTH, D_MODEL)),
        "w_in": w(ks[3], (DEPTH, D_MODEL, D_IN), D_MODEL),
        "w_alpha_up": w(ks[4], (DEPTH, GLA_RANK, GLA_KEY), GLA_RANK),
        "b_alpha": 0.1 * jax.random.normal(ks[5], (DEPTH, GLA_KEY), f32),
        "gla_head_norm": gain(ks[6], (DEPTH, GLA_HEADS, GLA_DV)),
        "mix_conv_w": w(ks[7], (DEPTH, CONV_K, CONV_DIM), CONV_K),
        "w_out": w(ks[8], (DEPTH, D_MIX, D_MODEL), D_MIX),
        "ffn_norm": gain(ks[9], (DEPTH, D_MODEL)),
        "w_up": w(ks[10], (DEPTH, D_MODEL, 2 * D_FF), D_MODEL),
        "ffn_conv_w": w(ks[11], (DEPTH, FFN_K, D_FF), FFN_K),
        "w_down": w(ks[12], (DEPTH, D_FF, D_MODEL), D_FF),
        "ple_norm": gain(ks[13], (DEPTH, D_MODEL)),
        "w_ple_gate": w(ks[14], (DEPTH, D_MODEL, D_MODEL), D_MODEL),
        "w_ple_proj": w(ks[15], (DEPTH, PLE_DIM, D_MODEL), PLE_DIM),
        "final_norm": gain(ks[16], (D_MODEL,)),
    }


def reference(x, p, attn_norm, w_in, w_alpha_up, b_alpha, gla_head_norm, mix_conv_w, w_out,
              ffn_norm, w_up, ffn_conv_w, w_down, ple_norm, w_ple_gate, w_ple_proj, final_norm):
    Bn, S, _ = x.shape
    split_idx = [int(i) for i in np.cumsum(IN_SPLIT_SIZES)[:-1]]
    h = x
    for i in range(DEPTH):
        y = rmsnorm(h, attn_norm[i])
        z = y @ w_in[i]
        q, k, v, g, a_low, cb, cc, cx = jnp.split(z, split_idx, axis=-1)

        a_logit = (a_low @ w_alpha_up[i] + b_alpha[i]).astype(jnp.float32)
        log_a = jax.nn.log_sigmoid(a_logit) / GLA_TAU
        o = gla_chunked(q.reshape(Bn, S, GLA_HEADS, GLA_DK),
                        k.reshape(Bn, S, GLA_HEADS, GLA_DK),
                        v.reshape(Bn, S, GLA_HEADS, GLA_DV),
                        log_a.reshape(Bn, S, GLA_HEADS, GLA_DK))
        o = rmsnorm(o, gla_head_norm[i]).astype(h.dtype)
        o = o * jax.nn.silu(g.reshape(Bn, S, GLA_HEADS, GLA_DV))
        gla_out = o.reshape(Bn, S, GLA_VAL)

        conv_out = cb * causal_dwconv(cc * cx, mix_conv_w[i])

        mix = jnp.concatenate([gla_out, conv_out], axis=-1)
        h = h + mix @ w_out[i]

        y = rmsnorm(h, ffn_norm[i])
        gate, up = jnp.split(y @ w_up[i], 2, axis=-1)
        gate = causal_dwconv(gate, ffn_conv_w[i])
        h = h + (jax.nn.silu(gate) * up) @ w_down[i]

        y = rmsnorm(h, ple_norm[i])
        ple_gate = jax.nn.sigmoid(y @ w_ple_gate[i])
        h = h + ple_gate * (p[i].astype(h.dtype) @ w_ple_proj[i])
    return rmsnorm(h, final_norm)
```

```python
import numpy as np
from contextlib import ExitStack
import concourse.bass as bass
import concourse.mybir as mybir
from concourse.bass_utils import run_bass_kernel_spmd

F32 = mybir.dt.float32
BF16 = mybir.dt.bfloat16
AF = mybir.ActivationFunctionType
ALU = mybir.AluOpType

ENGS = ("pe", "act", "dve", "pool", "sp")
SAME_ENG_RAW_WINDOW = 4
STRICT_SAME_ENGINE = True

D = 1024
KC = 8
DIN = 3088
DFF = 2816
NJ = 22
PLE = 256
EPS = 1e-6
SEQ_HALF = 4096


class Op:
    __slots__ = ("eng", "fn", "reads", "writes", "dsem", "idx", "waits", "inc", "val", "pos")


class Prog:
    def __init__(self):
        self.ops = []

    def add(self, eng, fn, reads=(), writes=(), dsem=None):
        o = Op()
        o.eng = eng
        o.fn = fn
        o.reads = tuple(reads)
        o.writes = tuple(writes)
        o.dsem = dsem
        o.idx = len(self.ops)
        o.waits = {}
        o.inc = False
        o.val = None
        o.pos = None
        self.ops.append(o)
        return o

    def analyze(self):
        last_w = {}
        readers = {}
        pos = {e: 0 for e in ENGS}
        dcount = {}
        deps_of = []
        for o in self.ops:
            o.pos = pos[o.eng]
            pos[o.eng] += 1
            if o.dsem is not None:
                dcount[o.dsem] = dcount.get(o.dsem, 0) + 16
                o.val = dcount[o.dsem]
            deps = {}
            for b in o.reads:
                for w in last_w.get(b, ()):
                    deps[w.idx] = (w, "raw")
            for b in o.writes:
                for w in last_w.get(b, ()):
                    if o.dsem is not None and w.dsem == o.dsem and not readers.get(b):
                        continue
                    if w.idx not in deps:
                        deps[w.idx] = (w, "waw")
                for r in readers.get(b, ()):
                    if r.idx not in deps:
                        deps[r.idx] = (r, "war")
            keep = []
            for w, kind in deps.values():
                if w is o:
                    continue
                if w.dsem is None and o.dsem is None and w.eng == o.eng:
                    if w.eng == "pe":
                        continue
                    if not STRICT_SAME_ENGINE:
                        if kind != "raw":
                            continue
                        if o.pos - w.pos > SAME_ENG_RAW_WINDOW:
                            continue
                keep.append(w)
            best = {}
            rest = []
            for w in keep:
                if w.dsem is None:
                    if w.eng not in best or best[w.eng].pos < w.pos:
                        best[w.eng] = w
                else:
                    rest.append(w)
            keep = rest + list(best.values())
            deps_of.append(keep)
            for b in o.reads:
                readers.setdefault(b, []).append(o)
            for b in o.writes:
                lw = last_w.get(b)
                if o.dsem is not None and lw and lw[0].dsem == o.dsem and not readers.get(b):
                    lw.append(o)
                else:
                    last_w[b] = [o]
                readers[b] = []
        for keep in deps_of:
            for w in keep:
                if w.dsem is None:
                    w.inc = True
        cnt = {e: 0 for e in ENGS}
        for o in self.ops:
            if o.dsem is None and o.inc:
                cnt[o.eng] += 1
                o.val = cnt[o.eng]
        waited = {e: {} for e in ENGS}
        nw = 0
        for o, keep in zip(self.ops, deps_of):
            need = {}
            for w in keep:
                key = w.dsem if w.dsem is not None else ("eng", w.eng)
                need[key] = max(need.get(key, 0), w.val)
            wd = waited[o.eng]
            for key, v in need.items():
                if wd.get(key, 0) >= v:
                    continue
                wd[key] = v
                o.waits[key] = v
                nw += 1
        self.stats = dict(nops=len(self.ops), nwaits=nw, incs=dict(cnt), dma=dict(dcount))
        return self.stats

    def emit(self, block, sems):
        by_eng = {e: [o for o in self.ops if o.eng == e] for e in ENGS}

        def body(ename):
            def run(eng):
                for o in by_eng[ename]:
                    for key, v in o.waits.items():
                        eng.wait_ge(sems[key], v)
                    if o.fn is None:
                        continue
                    ins = o.fn(eng)
                    if o.dsem is not None:
                        ins.then_inc(sems[o.dsem], 16)
                    elif o.inc:
                        ins.then_inc(sems[("eng", ename)], 1)
            return run

        block.tensor(body("pe"))
        block.scalar(body("act"))
        block.vector(body("dve"))
        block.gpsimd(body("pool"))
        block.sync(body("sp"))

    def sem_keys(self):
        keys = [("eng", e) for e in ENGS if e != "sp"]
        seen = set()
        for o in self.ops:
            if o.dsem is not None and o.dsem not in seen:
                seen.add(o.dsem)
                keys.append(o.dsem)
        return keys


def sl(s, n=128):
    return slice(s * n, (s + 1) * n)


def build_nc(NL=31, NF=8, HALO=True):
    NPRE = NL * 128 + (128 if HALO else 0)
    NTOK = NF * 512
    nc = bass.Bass("TRN2", target_bir_lowering=False)

    def din(name, shape):
        return nc.dram_tensor(name, list(shape), F32, kind="ExternalInput").ap()

    xp_d = din("xp", [max(NPRE, 128), D])
    x_d = din("x", [NTOK, D])
    p_d = din("p", [NTOK, PLE])
    attn_norm = din("attn_norm", [D])
    w_in = din("w_in", [D, DIN])
    w_alpha_up = din("w_alpha_up", [16, 256])
    b_alpha = din("b_alpha", [256])
    gla_head_norm = din("gla_head_norm", [4, 128])
    mix_conv_w = din("mix_conv_w", [3, 512])
    w_out = din("w_out", [D, D])
    ffn_norm = din("ffn_norm", [D])
    w_up = din("w_up", [D, 2 * DFF])
    ffn_conv_w = din("ffn_conv_w", [3, DFF])
    w_down = din("w_down", [DFF, D])
    ple_norm = din("ple_norm", [D])
    w_ple_gate = din("w_ple_gate", [D, D])
    w_ple_proj = din("w_ple_proj", [PLE, D])
    final_norm = din("final_norm", [D])
    out_d = nc.dram_tensor("out", [NTOK, D], F32, kind="ExternalOutput").ap()

    P = Prog()
    es = ExitStack()

    def sb(name, shape, dt):
        return es.enter_context(nc.sbuf_tensor(name, list(shape), dt))

    hT = [sb(f"h{i}", [128, 4, D], F32) for i in range(2)]
    ybfT = [[sb(f"ybf{p}_{i}", [128, D], BF16) for i in range(2)] for p in range(2)]
    junk = sb("junk", [128, D], BF16)
    ssT = [sb(f"ss{i}", [128, 4], F32) for i in range(2)]
    rsT = [sb(f"rs{i}", [128, 4], F32) for i in range(2)]
    rs2T = [sb(f"rs2_{i}", [128, 4], F32) for i in range(2)]
    yTT = [sb(f"yT{i}", [128, KC, 512], BF16) for i in range(2)]
    mixT = sb("mixT", [128, KC, 512], BF16)
    big = sb("big", [128, NJ * 256], F32)
    actT = big[:].bitcast(BF16).rearrange("p (j t) -> p j t", j=NJ)
    vtok = sb("vtok", [128, 4, 512], BF16)
    sg = sb("sg", [128, 4, 512], BF16)
    qdz = [sb(f"qdz{i}", [128, 2, 512], BF16) for i in range(2)]
    hm = sb("hm", [128, 2], F32)
    kd = sb("kd", [128, 2, 512], BF16)
    eb = sb("eb", [128, 2, 512], F32)
    enb = sb("enb", [128, 2, 512], BF16)
    Lt = sb("Lt", [128, 4, 256], BF16)
    ex = [sb(f"ex{i}", [128, 256], F32) for i in range(2)]
    aaug = sb("aaug", [32, 512], BF16)
    kdtok = [sb(f"kdtok{i}", [128, 256], BF16) for i in range(4)]
    scm = [sb(f"scm{i}", [128, 512], BF16) for i in range(4)]
    oT = sb("oT", [128, 4, 512], F32)
    osq = sb("osq", [128, 4, 512], BF16)
    R = sb("R", [128, 2, 256], F32)
    Sbf = sb("Sbf", [128, 2, 256], BF16)
    dsv = sb("dsv", [128, 2], F32)
    ubuf = sb("ubuf", [128, 4, 514], F32)
    NTMP = 4
    tmp = [sb(f"tmp{i}", [128, 512], F32) for i in range(NTMP)]
    gbuf = [sb(f"gbuf{i}", [128, 514], F32) for i in range(3)]
    ghalo = sb("ghalo", [128, NJ, 2], F32)
    pbuf = sb("pbuf", [128, 4, PLE], F32)
    pbf = sb("pbf", [128, 4, PLE], BF16)
    pT = sb("pT", [128, 2, 512], BF16)
    NWS = 4
    wslot = [sb(f"wslot{i}", [128, KC, 512], BF16) for i in range(NWS)]
    wstage = [big[:, i * 1024:(i + 1) * 1024].rearrange("p (a b) -> p a b", a=2) for i in range(5)]
    onesf = sb("onesf", [128, 512], F32)
    ident = sb("ident", [128, 128], BF16)
    identf = sb("identf", [128, 128], F32)
    mtri4 = sb("mtri4", [128, 512], BF16)
    negf = sb("negf", [128, 128], F32)
    trineg = sb("trineg", [128, 128], BF16)
    onesdv = sb("onesdv", [128, 128], BF16)
    epsT = sb("epsT", [128, 1], F32)
    one1 = sb("one1", [128, 1], F32)
    craw = sb("craw", [128, 128], F32)
    cvec = sb("cvec", [128, 128], F32)
    gfin = sb("gfin", [128, D], F32)
    walpha = sb("walpha", [32, 256], BF16)

    bk = [es.enter_context(nc.psum_tensor(f"bk{i}", [128, 512], F32)) for i in range(8)]
    bkb = [b[:].bitcast(BF16) for b in bk]
    bank_ctr = [0]

    reserved = set()

    def nb():
        while True:
            i = bank_ctr[0] % 8
            bank_ctr[0] += 1
            if i not in reserved:
                return i

    def reserve(n):
        got = [nb() for _ in range(n)]
        reserved.update(got)
        return got

    def release(bs):
        reserved.difference_update(bs)

    tmp_ctr = [0]

    def nt():
        i = tmp_ctr[0] % NTMP
        tmp_ctr[0] += 1
        return i

    C_ATTN, C_FFN, C_PLE, C_HG, C_MCW, C_FCW = 0, 8, 16, 24, 28, 40

    P.add("pool", lambda e: e.memset(onesf[:], 1.0), writes=["onesf"])
    P.add("pool", lambda e: e.affine_select(out=ident[:], in_=onesf[:, 0:128], pattern=[[-1, 128]], compare_op=ALU.is_equal, fill=0.0, base=0, channel_multiplier=1), reads=["onesf"], writes=["ident"])
    P.add("pool", lambda e: e.affine_select(out=identf[:], in_=onesf[:, 0:128], pattern=[[-1, 128]], compare_op=ALU.is_equal, fill=0.0, base=0, channel_multiplier=1), reads=["onesf"], writes=["identf"])
    P.add("pool", lambda e: e.affine_select(out=mtri4[:].rearrange("p (h i) -> p h i", h=4), in_=onesf[:].rearrange("p (h i) -> p h i", h=4), pattern=[[0, 4], [1, 128]], compare_op=ALU.is_ge, fill=0.0, base=0, channel_multiplier=-1), reads=["onesf"], writes=["mtri4"])
    P.add("pool", lambda e: e.memset(negf[:], -0.0625), writes=["negf"])
    P.add("pool", lambda e: e.affine_select(out=trineg[:], in_=negf[:], pattern=[[1, 128]], compare_op=ALU.is_ge, fill=0.0, base=0, channel_multiplier=-1), reads=["negf"], writes=["trineg"])
    P.add("pool", lambda e: e.memset(onesdv[:], 1.0 / 128.0), writes=["onesdv"])
    P.add("pool", lambda e: e.memset(epsT[:], EPS), writes=["epsT"])
    P.add("pool", lambda e: e.memset(one1[:], 1.0), writes=["one1"])
    P.add("pool", lambda e: e.memset(aaug[:], 1.0), writes=["aaug"])
    P.add("pool", lambda e: e.memset(hm[:], 0.0), writes=["hm"])
    P.add("pool", lambda e: e.memset(hm[0:64, 0:1], 0.125), writes=["hm"])
    P.add("pool", lambda e: e.memset(hm[64:128, 1:2], 0.125), writes=["hm"])
    P.add("pool", lambda e: e.memset(R[:], 0.0), writes=[("R", 0), ("R", 1)])
    P.add("pool", lambda e: e.memset(Sbf[:], 0.0), writes=[("Sbf", 0), ("Sbf", 1)])
    P.add("pool", lambda e: e.memset(dsv[:], 1.0), writes=[("dsv", 0), ("dsv", 1)])
    P.add("pool", lambda e: e.memset(ubuf[:], 0.0), writes=[("ub", c) for c in range(4)] + ["ubh"])
    P.add("pool", lambda e: e.memset(ghalo[:], 0.0), writes=[("gh", j) for j in range(NJ)])
    P.add("pool", lambda e: e.memset(craw[:], 0.0), writes=["craw"])
    crows = [
        (C_ATTN, 8, attn_norm.rearrange("(c p) -> c p", p=128)),
        (C_FFN, 8, ffn_norm.rearrange("(c p) -> c p", p=128)),
        (C_PLE, 8, ple_norm.rearrange("(c p) -> c p", p=128)),
        (C_HG, 4, gla_head_norm),
        (C_MCW, 12, mix_conv_w.rearrange("k (c p) -> (k c) p", p=128)),
        (C_FCW, 66, ffn_conv_w.rearrange("k (j p) -> (k j) p", p=128)),
    ]
    for r0, n, src in crows:
        P.add("sp", lambda e, r0=r0, n=n, src=src: e.dma_start(out=craw[r0:r0 + n, :], in_=src), reads=[], writes=["craw"], dsem="d_c1")
    P.add("pe", lambda e: e.transpose(out=bk[0][:, 0:128], in_=craw[:, :], identity=identf[:]), reads=["craw", "identf"], writes=[("bk", 0)])
    P.add("dve", lambda e: e.tensor_copy(out=cvec[:], in_=bk[0][:, 0:128]), reads=[("bk", 0)], writes=["cvec"])
    P.add("pool", lambda e: e.memset(tmp[1][0:32, 0:256], 0.0), writes=["wa_st", ("tmp", 1)])
    P.add("sp", lambda e: e.dma_start(out=tmp[1][0:16, 0:256], in_=w_alpha_up), writes=["wa_st", ("tmp", 1)], dsem="d_c2")
    P.add("sp", lambda e: e.dma_start(out=tmp[1][16:17, 0:256], in_=b_alpha.rearrange("(o n) -> o n", o=1)), writes=["wa_st", ("tmp", 1)], dsem="d_c2")
    P.add("pool", lambda e: e.tensor_copy(out=walpha[:], in_=tmp[1][0:32, 0:256]), reads=["wa_st", ("tmp", 1)], writes=["walpha"])
    for nh in range(2):
        P.add("sp", lambda e, nh=nh: e.dma_start(out=tmp[2 * nh][0:1, :], in_=final_norm[sl(nh, 512)].rearrange("(o n) -> o n", o=1)), writes=[("gfrow", nh), ("tmp", 2 * nh)], dsem=f"d_c3{nh}")
    for nh in range(2):
        P.add("pe", lambda e, nh=nh: e.matmul(bk[1 + nh][:, 0:512], lhsT=onesf[0:1, 0:128], rhs=tmp[2 * nh][0:1, :], start=True, stop=True), reads=["onesf", ("gfrow", nh), ("tmp", 2 * nh)], writes=[("bk", 1 + nh)])
        P.add("dve", lambda e, nh=nh: e.tensor_copy(out=gfin[:, sl(nh, 512)], in_=bk[1 + nh][:, 0:512]), reads=[("bk", 1 + nh)], writes=["gfin"])
    bank_ctr[0] = 3

    wg = {}

    def defgroup(name, nk, width, srcs):
        scr = nc.dram_tensor(f"scr_{name}", [128, nk, width], BF16, kind="Internal").ap()
        wg[name] = dict(nk=nk, width=width, srcs=srcs, scr=scr, conv=False)

    w_in_v = w_in.rearrange("(kc p) n -> p kc n", p=128)
    in_cols = [("in0", 0, 512), ("in1", 512, 512), ("in2", 1024, 512), ("in3", 1536, 16),
               ("in4", 1552, 512), ("in5", 2064, 512), ("in6", 2576, 512)]
    for name, c0, w in in_cols:
        defgroup(name, 8, w, [(0, w, w_in_v[:, :, c0:c0 + w])])
    w_out_v = w_out.rearrange("(kc p) n -> p kc n", p=128)
    w_pg_v = w_ple_gate.rearrange("(kc p) n -> p kc n", p=128)
    w_pp_v = w_ple_proj.rearrange("(kc p) n -> p kc n", p=128)
    for nh in range(2):
        defgroup(f"out{nh}", 8, 512, [(0, 512, w_out_v[:, :, sl(nh, 512)])])
        defgroup(f"pg{nh}", 8, 512, [(0, 512, w_pg_v[:, :, sl(nh, 512)])])
        defgroup(f"pp{nh}", 2, 512, [(0, 512, w_pp_v[:, :, sl(nh, 512)])])
    w_up_v = w_up.rearrange("(kc p) n -> p kc n", p=128)
    for g in range(11):
        defgroup(f"up{g}", 8, 512, [(0, 256, w_up_v[:, :, 256 * g:256 * g + 256]),
                                    (256, 256, w_up_v[:, :, DFF + 256 * g:DFF + 256 * g + 256])])
    w_dn_v = w_down.rearrange("(j p) n -> p j n", p=128)
    DN_GROUPS = [(0, 8), (8, 16), (16, 22)]
    for nh in range(2):
        for jg, (j0, j1) in enumerate(DN_GROUPS):
            defgroup(f"dn{nh}_{jg}", j1 - j0, 512, [(0, 512, w_dn_v[:, j0:j1, sl(nh, 512)])])

    ws_ctr = [0]
    st_ctr = [0]

    def wst_ids(st):
        return [("wst", st)] + [("act", j) for j in range(4 * st, 4 * st + 4)]

    def stage_and_cast(g, dst_tile, dst_ids):
        nk, width = g["nk"], g["width"]
        for k0 in range(0, nk, 4):
            k1 = min(nk, k0 + 4)
            st = st_ctr[0] % 2
            st_ctr[0] += 1
            for (dc, w, src) in g["srcs"]:
                P.add("sp", lambda e, st=st, k0=k0, k1=k1, dc=dc, w=w, src=src: e.dma_start(out=wstage[st][:, 0:k1 - k0, dc:dc + w], in_=src[:, k0:k1, :]),
                      writes=wst_ids(st), dsem=f"d_st{st}")
            P.add("pool", lambda e, st=st, k0=k0, k1=k1, width=width: e.tensor_copy(out=dst_tile[:, k0:k1, 0:width], in_=wstage[st][:, 0:k1 - k0, 0:width]),
                  reads=wst_ids(st), writes=dst_ids)

    def to_scratch(name, src_tile, src_ids):
        g = wg[name]
        nk, width = g["nk"], g["width"]
        P.add("pool", lambda e, nk=nk, width=width, scr=g["scr"]: e.dma_start(out=scr, in_=src_tile[:, 0:nk, 0:width]),
              reads=src_ids, writes=[("scr", name)], dsem=f"d_scr_{name}")
        g["conv"] = True

    held = set()

    def rel_w(*slots):
        for sl_ in slots:
            held.discard(sl_)

    def get_w(name):
        g = wg[name]
        while True:
            slot = ws_ctr[0] % NWS
            ws_ctr[0] += 1
            if slot not in held:
                break
        held.add(slot)
        nk, width = g["nk"], g["width"]
        assert g["conv"], name
        if True:
            P.add("sp", lambda e, slot=slot, nk=nk, width=width, scr=g["scr"]: e.dma_start(out=wslot[slot][:, 0:nk, 0:width], in_=scr),
                  reads=[("scr", name, k0) for k0 in range(0, nk, 2)], writes=[("ws", slot)], dsem=f"d_ws{slot}")
        return slot

    BG_ORDER = (["in1", "in3", "in0", "in2", "in5", "in6", "in4", "out0", "out1"] + [f"up{g}" for g in range(11)]
                + [f"dn{nh}_{jg}" for nh in range(2) for jg in range(3)] + ["pg0", "pp0", "pg1", "pp1"])
    NST = 5
    LOOK = 4

    def conv_gen():
        items = []
        for name in BG_ORDER:
            g = wg[name]
            for k0 in range(0, g["nk"], 2):
                items.append((name, k0, min(g["nk"], k0 + 2)))

        def load(i):
            name, k0, k1 = items[i]
            st = i % NST
            for (dc, w, src) in wg[name]["srcs"]:
                P.add("sp", lambda e, st=st, k0=k0, k1=k1, dc=dc, w=w, src=src: e.dma_start(out=wstage[st][:, 0:k1 - k0, dc:dc + w], in_=src[:, k0:k1, :]),
                      writes=wst_ids(st), dsem=f"d_st{st}")

        for i in range(min(LOOK, len(items))):
            load(i)
        for i, (name, k0, k1) in enumerate(items):
            st = i % NST
            q = i % 4
            width = wg[name]["width"]
            mids = [("mixT", 2 * q), ("mixT", 2 * q + 1)]
            P.add("pool", lambda e, st=st, k0=k0, k1=k1, q=q, width=width: e.tensor_copy(out=mixT[:, 2 * q:2 * q + k1 - k0, 0:width], in_=wstage[st][:, 0:k1 - k0, 0:width]),
                  reads=wst_ids(st), writes=mids)
            P.add("sp", lambda e, k0=k0, k1=k1, q=q, width=width, scr=wg[name]["scr"]: e.dma_start(out=scr[:, k0:k1, :], in_=mixT[:, 2 * q:2 * q + k1 - k0, 0:width]),
                  reads=mids, writes=[("scr", name, k0)], dsem=f"d_sw{q}")
            if i + LOOK < len(items):
                load(i + LOOK)
            if k1 == wg[name]["nk"]:
                wg[name]["conv"] = True
                yield "c"

    def cv(col):
        return cvec[:, col:col + 1]

    def rms_stats(hb, NS):
        h, ss, rs = hT[hb], ssT[hb], rsT[hb]
        for s in range(NS):
            P.add("act", lambda e, s=s: e.activation(out=junk[:], in_=h[:, s, :], func=AF.Square, accum_out=ss[:, s:s + 1]),
                  reads=[("h", hb, s)], writes=["junk", ("ss", hb, s)])
        P.add("dve", lambda e: e.tensor_scalar(out=rs[:, 0:NS], in0=ss[:, 0:NS], scalar1=1.0 / D, scalar2=EPS, op0=ALU.mult, op1=ALU.add),
              reads=[("ss", hb, s) for s in range(NS)], writes=[("rs", hb)])
        P.add("act", lambda e: e.activation(out=rs[:, 0:NS], in_=rs[:, 0:NS], func=AF.Sqrt), reads=[("rs", hb)], writes=[("rs", hb)])
        P.add("dve", lambda e: e.reciprocal(out=rs[:, 0:NS], in_=rs[:, 0:NS]), reads=[("rs", hb)], writes=[("rs", hb)])

    def norm_T(hb, NS, gcol):
        h, rs, yT, ybf = hT[hb], rsT[hb], yTT[hb], ybfT[hb]
        rms_stats(hb, NS)

        def scale(s):
            yb = s % 2
            P.add("act", lambda e, s=s, yb=yb: e.activation(out=ybf[yb][:], in_=h[:, s, :], func=AF.Copy, scale=rs[:, s:s + 1]),
                  reads=[("h", hb, s), ("rs", hb)], writes=[("ybf", hb, yb)])

        scale(0)
        yield "n"
        for s in range(NS):
            yb = s % 2
            b = nb()
            for kc in range(KC):
                P.add("pe", lambda e, b=b, kc=kc, yb=yb: e.transpose(out=bkb[b][:, sl(kc)], in_=ybf[yb][:, sl(kc)], identity=ident[:]),
                      reads=[("ybf", hb, yb), "ident"], writes=[("bk", b)])
            if s + 1 < NS:
                scale(s + 1)
            for kc in range(KC):
                if s % 2 == 0:
                    P.add("dve", lambda e, b=b, kc=kc, s=s: e.tensor_scalar(out=yT[:, kc, sl(s)], in0=bkb[b][:, sl(kc)], scalar1=cv(gcol + kc), scalar2=None, op0=ALU.mult),
                          reads=[("bk", b), "cvec"], writes=[("yT", hb, s)])
                else:
                    P.add("act", lambda e, b=b, kc=kc, s=s: e.activation(out=yT[:, kc, sl(s)], in_=bkb[b][:, sl(kc)], func=AF.Copy, scale=cv(gcol + kc)),
                          reads=[("bk", b), "cvec"], writes=[("yT", hb, s)])
            yield "n"

    def yT_ids(hb, NS):
        return [("yT", hb, s) for s in range(NS)]

    def proj_fm(hb, ws, c0, Tt, NS):
        yT = yTT[hb]
        b = nb()
        for kc in range(KC):
            P.add("pe", lambda e, b=b, kc=kc: e.matmul(bk[b][:, 0:Tt], lhsT=wslot[ws][:, kc, c0:c0 + 128], rhs=yT[:, kc, 0:Tt], start=(kc == 0), stop=(kc == KC - 1)),
                  reads=[("ws", ws)] + yT_ids(hb, NS), writes=[("bk", b)])
        return b

    def resid_add(hb, b, s, nh):
        h = hT[hb]
        P.add("dve", lambda e: e.tensor_tensor(out=h[:, s, sl(nh, 512)], in0=h[:, s, sl(nh, 512)], in1=bk[b][:, 0:512], op=ALU.add),
              reads=[("h", hb, s), ("bk", b)], writes=[("h", hb, s)])

    def tile(mode, src, r0, NS, hb, out_r0=None):
        Tt = NS * 128
        full = mode == "full"
        light = mode == "light"
        h, yT, rs = hT[hb], yTT[hb], rsT[hb]
        P.add("sp", lambda e: e.dma_start(out=h[:, 0:NS, :], in_=src[r0:r0 + Tt, :].rearrange("(s p) d -> p s d", p=128)),
              writes=[("h", hb, s) for s in range(NS)], dsem=f"d_x{hb}")
        yield from norm_T(hb, NS, C_ATTN)
        yield "A"
        ws = get_w("in1")
        for s in range(NS):
            b = nb()
            for kc in range(KC):
                P.add("pe", lambda e, b=b, kc=kc, s=s, ws=ws: e.matmul(bk[b][:, 0:512], lhsT=yT[:, kc, sl(s)], rhs=wslot[ws][:, kc, 0:512], start=(kc == 0), stop=(kc == KC - 1)),
                      reads=[("ws", ws), ("yT", hb, s)], writes=[("bk", b)])
            P.add("act", lambda e, b=b, s=s: e.activation(out=vtok[:, s, :], in_=bk[b][:, 0:512], func=AF.Copy),
                  reads=[("bk", b)], writes=[("vtok", s)])
            yield "c"
        rel_w(ws)
        if not light:
            ws = get_w("in2")
            for c in range(4):
                b = proj_fm(hb, ws, c * 128, Tt, NS)
                P.add("act", lambda e, b=b, c=c: e.activation(out=sg[:, c, 0:Tt], in_=bk[b][:, 0:Tt], func=AF.Silu),
                      reads=[("bk", b)], writes=[("sg", c)])
                yield "c"
        if not light:
            rel_w(ws)
        ws = get_w("in3")
        b = nb()
        for kc in range(KC):
            P.add("pe", lambda e, b=b, kc=kc, ws=ws: e.matmul(bk[b][0:16, 0:Tt], lhsT=wslot[ws][:, kc, 0:16], rhs=yT[:, kc, 0:Tt], start=(kc == 0), stop=(kc == KC - 1)),
                  reads=[("ws", ws)] + yT_ids(hb, NS), writes=[("bk", b)])
        P.add("dve", lambda e, b=b: e.tensor_copy(out=aaug[0:16, 0:Tt], in_=bk[b][0:16, 0:Tt]), reads=[("bk", b)], writes=["aaug"])
        rel_w(ws)
        yield "c"
        for s in range(NS):
            b = nb()
            P.add("pe", lambda e, b=b, s=s: e.matmul(bk[b][:, 0:256], lhsT=aaug[0:17, sl(s)], rhs=walpha[0:17, :], start=True, stop=True),
                  reads=["aaug", "walpha"], writes=[("bk", b)])
            xi = s % 2
            P.add("act", lambda e, b=b, xi=xi: e.activation(out=ex[xi][:], in_=bk[b][:, 0:256], func=AF.Exp, scale=-1.0),
                  reads=[("bk", b)], writes=[("ex", xi)])
            P.add("act", lambda e, s=s, xi=xi: e.activation(out=Lt[:, s, :], in_=ex[xi][:], func=AF.Ln, bias=one1[:, 0:1]),
                  reads=[("ex", xi), "one1"], writes=[("Lt", s)])
            if s % 2 == 1:
                yield "c"
        yield "c"
        for c in range(2):
            b = nb()
            for s in range(NS):
                P.add("pe", lambda e, b=b, s=s, c=c: e.matmul(bk[b][:, sl(s)], lhsT=Lt[:, s, sl(c)], rhs=trineg[:], start=True, stop=True),
                      reads=[("Lt", s), "trineg"], writes=[("bk", b)])
            P.add("act", lambda e, b=b, c=c: e.activation(out=eb[:, c, 0:Tt], in_=bk[b][:, 0:Tt], func=AF.Exp),
                  reads=[("bk", b)], writes=[("eb", c)])
            P.add("act", lambda e, b=b, c=c: e.activation(out=enb[:, c, 0:Tt], in_=bk[b][:, 0:Tt], func=AF.Exp, scale=-1.0),
                  reads=[("bk", b)], writes=[("enb", c)])
            yield "c"
        ws = get_w("in0")
        for c in range(2):
            if not light:
                b = proj_fm(hb, ws, c * 128, Tt, NS)
                for hh in range(2):
                    P.add("dve", lambda e, b=b, c=c, hh=hh: e.scalar_tensor_tensor(out=qdz[hh][:, c, 0:Tt], in0=bk[b][:, 0:Tt], scalar=hm[:, hh:hh + 1], in1=eb[:, c, 0:Tt], op0=ALU.mult, op1=ALU.mult),
                          reads=[("bk", b), ("eb", c), "hm"], writes=[("qd", c, hh)])
                yield "c"
            b = proj_fm(hb, ws, 256 + c * 128, Tt, NS)
            P.add("dve", lambda e, b=b, c=c: e.tensor_tensor(out=kd[:, c, 0:Tt], in0=bk[b][:, 0:Tt], in1=enb[:, c, 0:Tt], op=ALU.mult),
                  reads=[("bk", b), ("enb", c)], writes=[("kd", c)])
            yield "c"
        rel_w(ws)
        for s in range(NS):
            b = nb()
            for c in range(2):
                P.add("pe", lambda e, b=b, c=c, s=s: e.transpose(out=bkb[b][:, sl(c)], in_=kd[:, c, sl(s)], identity=ident[:]),
                      reads=[("kd", c), "ident"], writes=[("bk", b)])
            P.add("dve", lambda e, b=b, s=s: e.tensor_copy(out=kdtok[s][:], in_=bkb[b][:, 0:256]), reads=[("bk", b)], writes=[("kdtok", s)])
            if not light:
                b2 = nb()
                for hh4 in range(4):
                    c, hh = divmod(hh4, 2)
                    P.add("pe", lambda e, b2=b2, hh4=hh4, c=c, hh=hh, s=s: e.matmul(bk[b2][:, sl(hh4)], lhsT=kd[:, c, sl(s)], rhs=qdz[hh][:, c, sl(s)], start=True, stop=True),
                          reads=[("kd", c), ("qd", c, hh)], writes=[("bk", b2)])
                P.add("dve", lambda e, b2=b2, s=s: e.tensor_tensor(out=scm[s][:], in0=bk[b2][:, 0:512], in1=mtri4[:], op=ALU.mult),
                      reads=[("bk", b2), "mtri4"], writes=[("scm", s)])
            yield "c"
        for s in range(NS):
            if not light:
                b3 = nb()
                for hh4 in range(4):
                    c, hh = divmod(hh4, 2)
                    P.add("pe", lambda e, b3=b3, hh4=hh4, s=s: e.matmul(bk[b3][:, sl(hh4)], lhsT=vtok[:, s, sl(hh4)], rhs=scm[s][:, sl(hh4)], start=True, stop=False),
                          reads=[("vtok", s), ("scm", s)], writes=[("bk", b3)])
                    P.add("pe", lambda e, b3=b3, hh4=hh4, c=c, hh=hh, s=s: e.matmul(bk[b3][:, sl(hh4)], lhsT=Sbf[:, c, sl(hh)], rhs=qdz[hh][:, c, sl(s)], start=False, stop=True),
                          reads=[("Sbf", c), ("qd", c, hh)], writes=[("bk", b3)])
                P.add("act", lambda e, b3=b3, s=s: e.activation(out=oT[:, :, sl(s)], in_=bk[b3][:, 0:512].rearrange("p (a i) -> p a i", a=4), func=AF.Copy),
                      reads=[("bk", b3)], writes=[("oT", a) for a in range(4)])
                yield "c"
            for c in range(2):
                b4 = nb()
                P.add("pe", lambda e, b4=b4, c=c, s=s: e.matmul(bk[b4][:, 0:256], lhsT=kdtok[s][:, sl(c)], rhs=vtok[:, s, sl(c, 256)], start=True, stop=True),
                      reads=[("kdtok", s), ("vtok", s)], writes=[("bk", b4)])
                P.add("dve", lambda e, b4=b4, c=c: e.scalar_tensor_tensor(out=R[:, c, :], in0=R[:, c, :], scalar=dsv[:, c:c + 1], in1=bk[b4][:, 0:256], op0=ALU.mult, op1=ALU.add),
                      reads=[("R", c), ("dsv", c), ("bk", b4)], writes=[("R", c)])
                col = s * 128 + 127
                P.add("dve", lambda e, c=c, col=col: e.tensor_copy(out=dsv[:, c:c + 1], in_=eb[:, c, col:col + 1]),
                      reads=[("eb", c)], writes=[("dsv", c)])
                P.add("act", lambda e, c=c, col=col: e.activation(out=Sbf[:, c, :], in_=R[:, c, :], func=AF.Copy, scale=eb[:, c, col:col + 1]),
                      reads=[("R", c), ("eb", c)], writes=[("Sbf", c)])
            yield "c"
        if light:
            yield "M"
            return
        yield "H"
        for a in range(4):
            P.add("act", lambda e, a=a: e.activation(out=osq[:, a, 0:Tt], in_=oT[:, a, 0:Tt], func=AF.Square),
                  reads=[("oT", a)], writes=[("osq", a)])
        yield "c"
        hn = []
        for a in range(4):
            b = nb()
            P.add("pe", lambda e, b=b, a=a: e.matmul(bk[b][:, 0:Tt], lhsT=onesdv[:], rhs=osq[:, a, 0:Tt], start=True, stop=True),
                  reads=["onesdv", ("osq", a)], writes=[("bk", b)])
            t1 = nt()
            P.add("act", lambda e, b=b, t1=t1: e.activation(out=tmp[t1][:, 0:Tt], in_=bk[b][:, 0:Tt], func=AF.Sqrt, bias=epsT[:, 0:1]),
                  reads=[("bk", b), "epsT"], writes=[("tmp", t1)])
            P.add("dve", lambda e, t1=t1: e.reciprocal(out=tmp[t1][:, 0:Tt], in_=tmp[t1][:, 0:Tt]), reads=[("tmp", t1)], writes=[("tmp", t1)])
            P.add("dve", lambda e, t1=t1, a=a: e.tensor_tensor(out=tmp[t1][:, 0:Tt], in0=oT[:, a, 0:Tt], in1=tmp[t1][:, 0:Tt], op=ALU.mult),
                  reads=[("tmp", t1), ("oT", a)], writes=[("tmp", t1)])
            P.add("dve", lambda e, t1=t1, a=a: e.scalar_tensor_tensor(out=mixT[:, a, 0:Tt], in0=tmp[t1][:, 0:Tt], scalar=cv(C_HG + a), in1=sg[:, a, 0:Tt], op0=ALU.mult, op1=ALU.mult),
                  reads=[("tmp", t1), "cvec", ("sg", a)], writes=[("mixT", a)])
            yield "c"
        if not light:
            ws = get_w("in5")
            for c in range(4):
                b = proj_fm(hb, ws, c * 128, Tt, NS)
                P.add("act", lambda e, b=b, c=c: e.activation(out=oT[:, c, 0:Tt], in_=bk[b][:, 0:Tt], func=AF.Copy),
                      reads=[("bk", b)], writes=[("oT", c)])
                yield "c"
            rel_w(ws)
            ws = get_w("in6")
            for c in range(4):
                b = proj_fm(hb, ws, c * 128, Tt, NS)
                P.add("dve", lambda e, b=b, c=c: e.tensor_tensor(out=ubuf[:, c, 2:2 + Tt], in0=bk[b][:, 0:Tt], in1=oT[:, c, 0:Tt], op=ALU.mult),
                      reads=[("bk", b), ("oT", c)], writes=[("ub", c)])
                yield "c"
            rel_w(ws)
            ws = get_w("in4")
            for c in range(4):
                b = proj_fm(hb, ws, c * 128, Tt, NS)
                t1 = nt()
                P.add("act", lambda e, c=c, t1=t1: e.activation(out=tmp[t1][:, 0:Tt], in_=ubuf[:, c, 0:Tt], func=AF.Copy, scale=cv(C_MCW + 0 * 4 + c)),
                      reads=[("ub", c), "ubh", "cvec"], writes=[("tmp", t1)])
                P.add("dve", lambda e, c=c, t1=t1: e.scalar_tensor_tensor(out=tmp[t1][:, 0:Tt], in0=ubuf[:, c, 1:1 + Tt], scalar=cv(C_MCW + 1 * 4 + c), in1=tmp[t1][:, 0:Tt], op0=ALU.mult, op1=ALU.add),
                      reads=[("ub", c), "ubh", "cvec", ("tmp", t1)], writes=[("tmp", t1)])
                P.add("dve", lambda e, c=c, t1=t1: e.scalar_tensor_tensor(out=tmp[t1][:, 0:Tt], in0=ubuf[:, c, 2:2 + Tt], scalar=cv(C_MCW + 2 * 4 + c), in1=tmp[t1][:, 0:Tt], op0=ALU.mult, op1=ALU.add),
                      reads=[("ub", c), "cvec", ("tmp", t1)], writes=[("tmp", t1)])
                P.add("dve", lambda e, c=c, t1=t1, b=b: e.tensor_tensor(out=mixT[:, 4 + c, 0:Tt], in0=tmp[t1][:, 0:Tt], in1=bk[b][:, 0:Tt], op=ALU.mult),
                      reads=[("tmp", t1), ("bk", b)], writes=[("mixT", 4 + c)])
                yield "c"
            rel_w(ws)
            P.add("pool", lambda e: e.tensor_copy(out=ubuf[:, :, 0:2], in_=ubuf[:, :, Tt:Tt + 2]),
                  reads=[("ub", c) for c in range(4)], writes=["ubh"])
        yield "M"
        ws0 = get_w("out0")
        ws1 = get_w("out1")
        ybf, ss2, rs2 = ybfT[hb], ssT[hb], rs2T[hb]

        def wout(s):
            for nh, ws in ((0, ws0), (1, ws1)):
                b = nb()
                for kc in range(KC):
                    P.add("pe", lambda e, b=b, kc=kc, s=s, ws=ws: e.matmul(bk[b][:, 0:512], lhsT=mixT[:, kc, sl(s)], rhs=wslot[ws][:, kc, 0:512], start=(kc == 0), stop=(kc == KC - 1)),
                          reads=[("ws", ws), ("mixT", kc)], writes=[("bk", b)])
                resid_add(hb, b, s, nh)

        def stats(s):
            P.add("act", lambda e, s=s: e.activation(out=junk[:], in_=h[:, s, :], func=AF.Square, accum_out=ss2[:, s:s + 1]),
                  reads=[("h", hb, s)], writes=["junk", ("ss", hb, s)])
            P.add("dve", lambda e, s=s: e.tensor_scalar(out=rs2[:, s:s + 1], in0=ss2[:, s:s + 1], scalar1=1.0 / D, scalar2=EPS, op0=ALU.mult, op1=ALU.add),
                  reads=[("ss", hb, s)], writes=[("rs2", hb, s)])
            P.add("act", lambda e, s=s: e.activation(out=rs2[:, s:s + 1], in_=rs2[:, s:s + 1], func=AF.Sqrt), reads=[("rs2", hb, s)], writes=[("rs2", hb, s)])
            P.add("dve", lambda e, s=s: e.reciprocal(out=rs2[:, s:s + 1], in_=rs2[:, s:s + 1]), reads=[("rs2", hb, s)], writes=[("rs2", hb, s)])
            yb = s % 2
            P.add("act", lambda e, s=s, yb=yb: e.activation(out=ybf[yb][:], in_=h[:, s, :], func=AF.Copy, scale=rs2[:, s:s + 1]),
                  reads=[("h", hb, s), ("rs2", hb, s)], writes=[("ybf", hb, yb)])

        def tev(s):
            yb = s % 2
            b = nb()
            for kc in range(KC):
                P.add("pe", lambda e, b=b, kc=kc, yb=yb: e.transpose(out=bkb[b][:, sl(kc)], in_=ybf[yb][:, sl(kc)], identity=ident[:]),
                      reads=[("ybf", hb, yb), "ident"], writes=[("bk", b)])
            for kc in range(KC):
                if s % 2 == 0:
                    P.add("dve", lambda e, b=b, kc=kc, s=s: e.tensor_scalar(out=yT[:, kc, sl(s)], in0=bkb[b][:, sl(kc)], scalar1=cv(C_FFN + kc), scalar2=None, op0=ALU.mult),
                          reads=[("bk", b), "cvec"], writes=[("yT", hb, s)])
                else:
                    P.add("act", lambda e, b=b, kc=kc, s=s: e.activation(out=yT[:, kc, sl(s)], in_=bkb[b][:, sl(kc)], func=AF.Copy, scale=cv(C_FFN + kc)),
                          reads=[("bk", b), "cvec"], writes=[("yT", hb, s)])

        for s in range(NS):
            if s >= 2:
                tev(s - 2)
            wout(s)
            stats(s)
            yield "c"
        rel_w(ws0, ws1)
        for s in range(max(0, NS - 2), NS):
            tev(s)
            yield "c"
        yield "W"
        if full:
            P.add("sp", lambda e: e.dma_start(out=pbuf[:, 0:NS, :], in_=p_d[r0:r0 + Tt, :].rearrange("(s p) d -> p s d", p=128)),
                  writes=["pbuf"], dsem="d_p")
            P.add("pool", lambda e: e.tensor_copy(out=pbf[:, 0:NS, :], in_=pbuf[:, 0:NS, :]), reads=["pbuf"], writes=["pbf"])
        ws = None
        for g in range(11):
            if ws is not None:
                rel_w(ws)
            ws = get_w(f"up{g}")
            for jj in range(2):
                j = 2 * g + jj
                bg = proj_fm(hb, ws, jj * 128, Tt, NS)
                gi = j % 3
                P.add("pool", lambda e, gi=gi, j=j: e.tensor_copy(out=gbuf[gi][:, 0:2], in_=ghalo[:, j, :]),
                      reads=[("gh", j)], writes=[("gbh", gi)])
                P.add("act", lambda e, gi=gi, bg=bg: e.activation(out=gbuf[gi][:, 2:2 + Tt], in_=bk[bg][:, 0:Tt], func=AF.Copy),
                      reads=[("bk", bg)], writes=[("gb", gi)])
                P.add("pool", lambda e, gi=gi, j=j: e.tensor_copy(out=ghalo[:, j, :], in_=gbuf[gi][:, Tt:Tt + 2]),
                      reads=[("gb", gi)], writes=[("gh", j)])
                if not full:
                    yield "c"
                    continue
                bu = proj_fm(hb, ws, 256 + jj * 128, Tt, NS)
                t1 = nt()
                P.add("act", lambda e, gi=gi, j=j, t1=t1: e.activation(out=tmp[t1][:, 0:Tt], in_=gbuf[gi][:, 0:Tt], func=AF.Copy, scale=cv(C_FCW + 0 * NJ + j)),
                      reads=[("gb", gi), ("gbh", gi), "cvec"], writes=[("tmp", t1)])
                P.add("dve", lambda e, gi=gi, j=j, t1=t1: e.scalar_tensor_tensor(out=tmp[t1][:, 0:Tt], in0=gbuf[gi][:, 1:1 + Tt], scalar=cv(C_FCW + 1 * NJ + j), in1=tmp[t1][:, 0:Tt], op0=ALU.mult, op1=ALU.add),
                      reads=[("gb", gi), ("gbh", gi), "cvec", ("tmp", t1)], writes=[("tmp", t1)])
                P.add("dve", lambda e, gi=gi, j=j, t1=t1: e.scalar_tensor_tensor(out=tmp[t1][:, 0:Tt], in0=gbuf[gi][:, 2:2 + Tt], scalar=cv(C_FCW + 2 * NJ + j), in1=tmp[t1][:, 0:Tt], op0=ALU.mult, op1=ALU.add),
                      reads=[("gb", gi), "cvec", ("tmp", t1)], writes=[("tmp", t1)])
                P.add("act", lambda e, t1=t1: e.activation(out=tmp[t1][:, 0:Tt], in_=tmp[t1][:, 0:Tt], func=AF.Silu),
                      reads=[("tmp", t1)], writes=[("tmp", t1)])
                P.add("dve", lambda e, t1=t1, j=j, bu=bu: e.tensor_tensor(out=actT[:, j, 0:Tt], in0=tmp[t1][:, 0:Tt], in1=bk[bu][:, 0:Tt], op=ALU.mult),
                      reads=[("tmp", t1), ("bk", bu)], writes=[("act", j)])
                yield "c"
        rel_w(ws)
        if not full:
            return
        for nh in range(2):
            banks4 = reserve(NS)
            for jg, (j0, j1) in enumerate(DN_GROUPS):
                ws = get_w(f"dn{nh}_{jg}")
                for s in range(NS):
                    for j in range(j0, j1):
                        P.add("pe", lambda e, b=banks4[s], j=j, j0=j0, s=s, ws=ws: e.matmul(bk[b][:, 0:512], lhsT=actT[:, j, sl(s)], rhs=wslot[ws][:, j - j0, 0:512], start=(j == 0), stop=(j == NJ - 1)),
                              reads=[("ws", ws), ("act", j)], writes=[("bk", banks4[s])])
                    yield "c"
                rel_w(ws)
            for s in range(NS):
                resid_add(hb, banks4[s], s, nh)
            release(banks4)
            yield "c"
        yield from norm_T(hb, NS, C_PLE)
        for s in range(NS):
            b = nb()
            for kc in range(2):
                P.add("pe", lambda e, b=b, kc=kc, s=s: e.transpose(out=bkb[b][:, sl(kc)], in_=pbf[:, s, sl(kc)], identity=ident[:]),
                      reads=["pbf", "ident"], writes=[("bk", b)])
            P.add("dve", lambda e, b=b, s=s: e.tensor_copy(out=pT[:, :, sl(s)], in_=bkb[b][:, 0:256].rearrange("p (a i) -> p a i", a=2)),
                  reads=[("bk", b)], writes=[("pT", s)])
        yield "c"
        for nh in range(2):
            wsg = get_w(f"pg{nh}")
            wsp = get_w(f"pp{nh}")
            for s in range(NS):
                bg = nb()
                for kc in range(KC):
                    P.add("pe", lambda e, bg=bg, kc=kc, s=s, wsg=wsg: e.matmul(bk[bg][:, 0:512], lhsT=yT[:, kc, sl(s)], rhs=wslot[wsg][:, kc, 0:512], start=(kc == 0), stop=(kc == KC - 1)),
                          reads=[("ws", wsg), ("yT", hb, s)], writes=[("bk", bg)])
                bp = nb()
                for kc in range(2):
                    P.add("pe", lambda e, bp=bp, kc=kc, s=s, wsp=wsp: e.matmul(bk[bp][:, 0:512], lhsT=pT[:, kc, sl(s)], rhs=wslot[wsp][:, kc, 0:512], start=(kc == 0), stop=(kc == 1)),
                          reads=[("ws", wsp), ("pT", s)], writes=[("bk", bp)])
                t1 = nt()
                P.add("act", lambda e, t1=t1, bg=bg: e.activation(out=tmp[t1][:], in_=bk[bg][:, 0:512], func=AF.Sigmoid),
                      reads=[("bk", bg)], writes=[("tmp", t1)])
                P.add("dve", lambda e, t1=t1, bp=bp: e.tensor_tensor(out=tmp[t1][:], in0=tmp[t1][:], in1=bk[bp][:, 0:512], op=ALU.mult),
                      reads=[("tmp", t1), ("bk", bp)], writes=[("tmp", t1)])
                P.add("pool", lambda e, t1=t1, s=s, nh=nh: e.tensor_tensor(out=h[:, s, sl(nh, 512)], in0=h[:, s, sl(nh, 512)], in1=tmp[t1][:], op=ALU.add),
                      reads=[("tmp", t1), ("h", hb, s)], writes=[("h", hb, s)])
                yield "c"
            rel_w(wsg, wsp)
        rms_stats(hb, NS)
        yield "c"
        for s in range(NS):
            P.add("dve", lambda e, s=s: e.scalar_tensor_tensor(out=h[:, s, :], in0=h[:, s, :], scalar=rs[:, s:s + 1], in1=gfin[:], op0=ALU.mult, op1=ALU.mult),
                  reads=[("h", hb, s), ("rs", hb), "gfin"], writes=[("h", hb, s)])
            P.add("pool", lambda e, s=s: e.dma_start(out=out_d[out_r0 + s * 128:out_r0 + (s + 1) * 128, :], in_=h[:, s, :]),
                  reads=[("h", hb, s)], writes=[("o", out_r0, s)], dsem=f"d_o{hb}")
            yield "c"

    specs = []
    r = 0
    nl = NL
    while nl > 0:
        ns = min(4, nl)
        specs.append(("light", xp_d, r, ns, None))
        r += ns * 128
        nl -= ns
    if HALO:
        specs.append(("halo", xp_d, r, 1, None))
    out_ids = []
    for t in range(NF):
        specs.append(("full", x_d, t * 512, 4, t * 512))
        out_ids += [("o", t * 512, s) for s in range(4)]
    runs = []
    for i, (mode, src, r0, ns, o0) in enumerate(specs):
        runs.append(dict(g=tile(mode, src, r0, ns, i % 2, out_r0=o0), tag=None, fin=False, mode=mode))
    bg = dict(g=conv_gen(), fin=False)

    def bg_step():
        if not bg["fin"]:
            try:
                next(bg["g"])
            except StopIteration:
                bg["fin"] = True

    def one(t):
        try:
            t["tag"] = next(t["g"])
        except StopIteration:
            t["fin"] = True
            t["tag"] = "END"

    def reached(t, targets):
        return t is None or t["fin"] or t["tag"] in targets

    nstep = [0]

    def interleave(a, ta, b, tb, ra=2, rb=1):
        while not (reached(a, ta) and reached(b, tb)):
            for _ in range(ra):
                if reached(a, ta):
                    break
                one(a)
            for _ in range(rb):
                if reached(b, tb):
                    break
                one(b)
            nstep[0] += 1
            if nstep[0] % 2 == 0:
                bg_step()

    def drain_bg():
        while not bg["fin"]:
            bg_step()

    if runs[0]["mode"] != "light":
        drain_bg()
    while not wg["in0"]["conv"]:
        bg_step()
    for i, cur in enumerate(runs):
        nxt = runs[i + 1] if i + 1 < len(runs) else None
        if nxt is not None and nxt["mode"] != "light":
            drain_bg()
        if cur["mode"] == "light":
            interleave(cur, {"W"}, nxt, {"A"})
        else:
            interleave(cur, {"W"}, None, {"A"})
        credit = 0.0
        while not reached(cur, {"END"}):
            one(cur)
            credit += 2.0 if cur["tag"] == "n" else 0.75
            while credit >= 1.0 and not reached(nxt, {"M"}):
                one(nxt)
                credit -= 1.0
            if reached(nxt, {"M"}):
                credit = 0.0
            nstep[0] += 1
            if nstep[0] % 2 == 0:
                bg_step()
    while not bg["fin"]:
        bg_step()
    P.add("sp", None, reads=out_ids)

    stats = P.analyze()
    keys = P.sem_keys()
    sems = {k: es.enter_context(nc.semaphore(f"s{i}")) for i, k in enumerate(keys)}
    with nc.Block() as block:
        P.emit(block, sems)
    es.close()
    return nc, stats


_NAMES = ["attn_norm", "w_in", "w_alpha_up", "b_alpha", "gla_head_norm", "mix_conv_w", "w_out", "ffn_norm",
          "w_up", "ffn_conv_w", "w_down", "ple_norm", "w_ple_gate", "w_ple_proj"]


def make_in_maps(inputs, n_cores=8):
    x = np.asarray(inputs["x"], dtype=np.float32)
    p = np.asarray(inputs["p"], dtype=np.float32)
    common = {k: np.ascontiguousarray(np.asarray(inputs[k], dtype=np.float32)[0]) for k in _NAMES}
    common["final_norm"] = np.ascontiguousarray(np.asarray(inputs["final_norm"], dtype=np.float32))
    maps = []
    for c in range(n_cores):
        b, hf = divmod(c, 2)
        m = dict(common)
        m["x"] = np.ascontiguousarray(x[b, hf * SEQ_HALF:(hf + 1) * SEQ_HALF])
        m["p"] = np.ascontiguousarray(p[0, b, hf * SEQ_HALF:(hf + 1) * SEQ_HALF])
        if hf == 1:
            m["xp"] = np.ascontiguousarray(x[b, 0:SEQ_HALF])
        else:
            m["xp"] = np.zeros((SEQ_HALF, D), np.float32)
        maps.append(m)
    return maps


def kernel(**inputs):
    nc, _ = build_nc()
    maps = make_in_maps(inputs)
    res = run_bass_kernel_spmd(nc, maps, core_ids=list(range(8)))
    x = inputs["x"]
    out = np.empty((4, 2 * SEQ_HALF, D), np.float32)
    for c in range(8):
        b, hf = divmod(c, 2)
        out[b, hf * SEQ_HALF:(hf + 1) * SEQ_HALF] = np.asarray(res.results[c]["out"], dtype=np.float32)
    return out
```

```python
import numpy as np
from contextlib import ExitStack
import concourse.bass as bass
import concourse.mybir as mybir
from concourse.bass_utils import run_bass_kernel_spmd

F32 = mybir.dt.float32
BF16 = mybir.dt.bfloat16
AF = mybir.ActivationFunctionType
ALU = mybir.AluOpType

ENGS = ("pe", "act", "dve", "pool", "sp")
SAME_ENG_RAW_WINDOW = 4
STRICT_SAME_ENGINE = True

D = 1024
KC = 8
DIN = 3088
DFF = 2816
NJ = 22
PLE = 256
EPS = 1e-6
SEQ_HALF = 4096


class Op:
    __slots__ = ("eng", "fn", "reads", "writes", "dsem", "idx", "waits", "inc", "val", "pos")


class Prog:
    def __init__(self):
        self.ops = []

    def add(self, eng, fn, reads=(), writes=(), dsem=None):
        o = Op()
        o.eng = eng
        o.fn = fn
        o.reads = tuple(reads)
        o.writes = tuple(writes)
        o.dsem = dsem
        o.idx = len(self.ops)
        o.waits = {}
        o.inc = False
        o.val = None
        o.pos = None
        self.ops.append(o)
        return o

    def analyze(self):
        last_w = {}
        readers = {}
        pos = {e: 0 for e in ENGS}
        dcount = {}
        deps_of = []
        for o in self.ops:
            o.pos = pos[o.eng]
            pos[o.eng] += 1
            if o.dsem is not None:
                dcount[o.dsem] = dcount.get(o.dsem, 0) + 16
                o.val = dcount[o.dsem]
            deps = {}
            for b in o.reads:
                for w in last_w.get(b, ()):
                    deps[w.idx] = (w, "raw")
            for b in o.writes:
                for w in last_w.get(b, ()):
                    if o.dsem is not None and w.dsem == o.dsem and not readers.get(b):
                        continue
                    if w.idx not in deps:
                        deps[w.idx] = (w, "waw")
                for r in readers.get(b, ()):
                    if r.idx not in deps:
                        deps[r.idx] = (r, "war")
            keep = []
            for w, kind in deps.values():
                if w is o:
                    continue
                if w.dsem is None and o.dsem is None and w.eng == o.eng:
                    if w.eng == "pe":
                        continue
                    if not STRICT_SAME_ENGINE:
                        if kind != "raw":
                            continue
                        if o.pos - w.pos > SAME_ENG_RAW_WINDOW:
                            continue
                keep.append(w)
            best = {}
            rest = []
            for w in keep:
                if w.dsem is None:
                    if w.eng not in best or best[w.eng].pos < w.pos:
                        best[w.eng] = w
                else:
                    rest.append(w)
            keep = rest + list(best.values())
            deps_of.append(keep)
            for b in o.reads:
                readers.setdefault(b, []).append(o)
            for b in o.writes:
                lw = last_w.get(b)
                if o.dsem is not None and lw and lw[0].dsem == o.dsem and not readers.get(b):
                    lw.append(o)
                else:
                    last_w[b] = [o]
                readers[b] = []
        for keep in deps_of:
            for w in keep:
                if w.dsem is None:
                    w.inc = True
        cnt = {e: 0 for e in ENGS}
        for o in self.ops:
            if o.dsem is None and o.inc:
                cnt[o.eng] += 1
                o.val = cnt[o.eng]
        waited = {e: {} for e in ENGS}
        nw = 0
        for o, keep in zip(self.ops, deps_of):
            need = {}
            for w in keep:
                key = w.dsem if w.dsem is not None else ("eng", w.eng)
                need[key] = max(need.get(key, 0), w.val)
            wd = waited[o.eng]
            for key, v in need.items():
                if wd.get(key, 0) >= v:
                    continue
                wd[key] = v
                o.waits[key] = v
                nw += 1
        self.stats = dict(nops=len(self.ops), nwaits=nw, incs=dict(cnt), dma=dict(dcount))
        return self.stats

    def emit(self, block, sems):
        by_eng = {e: [o for o in self.ops if o.eng == e] for e in ENGS}

        def body(ename):
            def run(eng):
                for o in by_eng[ename]:
                    for key, v in o.waits.items():
                        eng.wait_ge(sems[key], v)
                    if o.fn is None:
                        continue
                    ins = o.fn(eng)
                    if o.dsem is not None:
                        ins.then_inc(sems[o.dsem], 16)
                    elif o.inc:
                        ins.then_inc(sems[("eng", ename)], 1)
            return run

        block.tensor(body("pe"))
        block.scalar(body("act"))
        block.vector(body("dve"))
        block.gpsimd(body("pool"))
        block.sync(body("sp"))

    def sem_keys(self):
        keys = [("eng", e) for e in ENGS if e != "sp"]
        seen = set()
        for o in self.ops:
            if o.dsem is not None and o.dsem not in seen:
                seen.add(o.dsem)
                keys.append(o.dsem)
        return keys


def sl(s, n=128):
    return slice(s * n, (s + 1) * n)


def build_nc(NL=31, NF=8, HALO=True):
    NPRE = NL * 128 + (128 if HALO else 0)
    NTOK = NF * 512
    nc = bass.Bass("TRN2", target_bir_lowering=False)

    def din(name, shape):
        return nc.dram_tensor(name, list(shape), F32, kind="ExternalInput").ap()

    xp_d = din("xp", [max(NPRE, 128), D])
    x_d = din("x", [NTOK, D])
    p_d = din("p", [NTOK, PLE])
    attn_norm = din("attn_norm", [D])
    w_in = din("w_in", [D, DIN])
    w_alpha_up = din("w_alpha_up", [16, 256])
    b_alpha = din("b_alpha", [256])
    gla_head_norm = din("gla_head_norm", [4, 128])
    mix_conv_w = din("mix_conv_w", [3, 512])
    w_out = din("w_out", [D, D])
    ffn_norm = din("ffn_norm", [D])
    w_up = din("w_up", [D, 2 * DFF])
    ffn_conv_w = din("ffn_conv_w", [3, DFF])
    w_down = din("w_down", [DFF, D])
    ple_norm = din("ple_norm", [D])
    w_ple_gate = din("w_ple_gate", [D, D])
    w_ple_proj = din("w_ple_proj", [PLE, D])
    final_norm = din("final_norm", [D])
    out_d = nc.dram_tensor("out", [NTOK, D], F32, kind="ExternalOutput").ap()

    P = Prog()
    es = ExitStack()

    def sb(name, shape, dt):
        return es.enter_context(nc.sbuf_tensor(name, list(shape), dt))

    hT = [sb(f"h{i}", [128, 4, D], F32) for i in range(2)]
    ybfT = [[sb(f"ybf{p}_{i}", [128, D], BF16) for i in range(2)] for p in range(2)]
    junk = sb("junk", [128, D], BF16)
    ssT = [sb(f"ss{i}", [128, 4], F32) for i in range(2)]
    rsT = [sb(f"rs{i}", [128, 4], F32) for i in range(2)]
    yTT = [sb(f"yT{i}", [128, KC, 512], BF16) for i in range(2)]
    mixT = sb("mixT", [128, KC, 512], BF16)
    big = sb("big", [128, NJ * 256], F32)
    actT = big[:].bitcast(BF16).rearrange("p (j t) -> p j t", j=NJ)
    vtok = sb("vtok", [128, 4, 512], BF16)
    sg = sb("sg", [128, 4, 512], BF16)
    qdz = [sb(f"qdz{i}", [128, 2, 512], BF16) for i in range(2)]
    hm = sb("hm", [128, 2], F32)
    kd = sb("kd", [128, 2, 512], BF16)
    eb = sb("eb", [128, 2, 512], F32)
    enb = sb("enb", [128, 2, 512], BF16)
    Lt = sb("Lt", [128, 4, 256], BF16)
    ex = [sb(f"ex{i}", [128, 256], F32) for i in range(2)]
    aaug = sb("aaug", [32, 512], BF16)
    kdtok = [sb(f"kdtok{i}", [128, 256], BF16) for i in range(4)]
    scm = [sb(f"scm{i}", [128, 512], BF16) for i in range(4)]
    oT = sb("oT", [128, 4, 512], F32)
    osq = sb("osq", [128, 4, 512], BF16)
    R = sb("R", [128, 2, 256], F32)
    Sbf = sb("Sbf", [128, 2, 256], BF16)
    dsv = sb("dsv", [128, 2], F32)
    ubuf = sb("ubuf", [128, 4, 514], F32)
    NTMP = 4
    tmp = [sb(f"tmp{i}", [128, 512], F32) for i in range(NTMP)]
    gbuf = [sb(f"gbuf{i}", [128, 514], F32) for i in range(3)]
    ghalo = sb("ghalo", [128, NJ, 2], F32)
    pbuf = sb("pbuf", [128, 4, PLE], F32)
    pbf = sb("pbf", [128, 4, PLE], BF16)
    pT = sb("pT", [128, 2, 512], BF16)
    NWS = 4
    wslot = [sb(f"wslot{i}", [128, KC, 512], BF16) for i in range(NWS)]
    wstage = [big[:, i * 1024:(i + 1) * 1024].rearrange("p (a b) -> p a b", a=2) for i in range(5)]
    onesf = sb("onesf", [128, 512], F32)
    ident = sb("ident", [128, 128], BF16)
    identf = sb("identf", [128, 128], F32)
    mtri4 = sb("mtri4", [128, 512], BF16)
    negf = sb("negf", [128, 128], F32)
    trineg = sb("trineg", [128, 128], BF16)
    onesdv = sb("onesdv", [128, 128], BF16)
    epsT = sb("epsT", [128, 1], F32)
    one1 = sb("one1", [128, 1], F32)
    craw = sb("craw", [128, 128], F32)
    cvec = sb("cvec", [128, 128], F32)
    gfin = sb("gfin", [128, D], F32)
    walpha = sb("walpha", [32, 256], BF16)

    bk = [es.enter_context(nc.psum_tensor(f"bk{i}", [128, 512], F32)) for i in range(8)]
    bkb = [b[:].bitcast(BF16) for b in bk]
    bank_ctr = [0]

    reserved = set()

    def nb():
        while True:
            i = bank_ctr[0] % 8
            bank_ctr[0] += 1
            if i not in reserved:
                return i

    def reserve(n):
        got = [nb() for _ in range(n)]
        reserved.update(got)
        return got

    def release(bs):
        reserved.difference_update(bs)

    tmp_ctr = [0]

    def nt():
        i = tmp_ctr[0] % NTMP
        tmp_ctr[0] += 1
        return i

    C_ATTN, C_FFN, C_PLE, C_HG, C_MCW, C_FCW = 0, 8, 16, 24, 28, 40

    P.add("pool", lambda e: e.memset(onesf[:], 1.0), writes=["onesf"])
    P.add("pool", lambda e: e.affine_select(out=ident[:], in_=onesf[:, 0:128], pattern=[[-1, 128]], compare_op=ALU.is_equal, fill=0.0, base=0, channel_multiplier=1), reads=["onesf"], writes=["ident"])
    P.add("pool", lambda e: e.affine_select(out=identf[:], in_=onesf[:, 0:128], pattern=[[-1, 128]], compare_op=ALU.is_equal, fill=0.0, base=0, channel_multiplier=1), reads=["onesf"], writes=["identf"])
    P.add("pool", lambda e: e.affine_select(out=mtri4[:].rearrange("p (h i) -> p h i", h=4), in_=onesf[:].rearrange("p (h i) -> p h i", h=4), pattern=[[0, 4], [1, 128]], compare_op=ALU.is_ge, fill=0.0, base=0, channel_multiplier=-1), reads=["onesf"], writes=["mtri4"])
    P.add("pool", lambda e: e.memset(negf[:], -0.0625), writes=["negf"])
    P.add("pool", lambda e: e.affine_select(out=trineg[:], in_=negf[:], pattern=[[1, 128]], compare_op=ALU.is_ge, fill=0.0, base=0, channel_multiplier=-1), reads=["negf"], writes=["trineg"])
    P.add("pool", lambda e: e.memset(onesdv[:], 1.0 / 128.0), writes=["onesdv"])
    P.add("pool", lambda e: e.memset(epsT[:], EPS), writes=["epsT"])
    P.add("pool", lambda e: e.memset(one1[:], 1.0), writes=["one1"])
    P.add("pool", lambda e: e.memset(aaug[:], 1.0), writes=["aaug"])
    P.add("pool", lambda e: e.memset(hm[:], 0.0), writes=["hm"])
    P.add("pool", lambda e: e.memset(hm[0:64, 0:1], 0.125), writes=["hm"])
    P.add("pool", lambda e: e.memset(hm[64:128, 1:2], 0.125), writes=["hm"])
    P.add("pool", lambda e: e.memset(R[:], 0.0), writes=[("R", 0), ("R", 1)])
    P.add("pool", lambda e: e.memset(Sbf[:], 0.0), writes=[("Sbf", 0), ("Sbf", 1)])
    P.add("pool", lambda e: e.memset(dsv[:], 1.0), writes=[("dsv", 0), ("dsv", 1)])
    P.add("pool", lambda e: e.memset(ubuf[:], 0.0), writes=[("ub", c) for c in range(4)] + ["ubh"])
    P.add("pool", lambda e: e.memset(ghalo[:], 0.0), writes=[("gh", j) for j in range(NJ)])
    P.add("pool", lambda e: e.memset(craw[:], 0.0), writes=["craw"])
    crows = [
        (C_ATTN, 8, attn_norm.rearrange("(c p) -> c p", p=128)),
        (C_FFN, 8, ffn_norm.rearrange("(c p) -> c p", p=128)),
        (C_PLE, 8, ple_norm.rearrange("(c p) -> c p", p=128)),
        (C_HG, 4, gla_head_norm),
        (C_MCW, 12, mix_conv_w.rearrange("k (c p) -> (k c) p", p=128)),
        (C_FCW, 66, ffn_conv_w.rearrange("k (j p) -> (k j) p", p=128)),
    ]
    for r0, n, src in crows:
        P.add("sp", lambda e, r0=r0, n=n, src=src: e.dma_start(out=craw[r0:r0 + n, :], in_=src), reads=[], writes=["craw"], dsem="d_c1")
    P.add("pe", lambda e: e.transpose(out=bk[0][:, 0:128], in_=craw[:, :], identity=identf[:]), reads=["craw", "identf"], writes=[("bk", 0)])
    P.add("dve", lambda e: e.tensor_copy(out=cvec[:], in_=bk[0][:, 0:128]), reads=[("bk", 0)], writes=["cvec"])
    P.add("pool", lambda e: e.memset(tmp[1][0:32, 0:256], 0.0), writes=["wa_st", ("tmp", 1)])
    P.add("sp", lambda e: e.dma_start(out=tmp[1][0:16, 0:256], in_=w_alpha_up), writes=["wa_st", ("tmp", 1)], dsem="d_c2")
    P.add("sp", lambda e: e.dma_start(out=tmp[1][16:17, 0:256], in_=b_alpha.rearrange("(o n) -> o n", o=1)), writes=["wa_st", ("tmp", 1)], dsem="d_c2")
    P.add("pool", lambda e: e.tensor_copy(out=walpha[:], in_=tmp[1][0:32, 0:256]), reads=["wa_st", ("tmp", 1)], writes=["walpha"])
    for nh in range(2):
        P.add("sp", lambda e, nh=nh: e.dma_start(out=tmp[2 * nh][0:1, :], in_=final_norm[sl(nh, 512)].rearrange("(o n) -> o n", o=1)), writes=[("gfrow", nh), ("tmp", 2 * nh)], dsem=f"d_c3{nh}")
    for nh in range(2):
        P.add("pe", lambda e, nh=nh: e.matmul(bk[1 + nh][:, 0:512], lhsT=onesf[0:1, 0:128], rhs=tmp[2 * nh][0:1, :], start=True, stop=True), reads=["onesf", ("gfrow", nh), ("tmp", 2 * nh)], writes=[("bk", 1 + nh)])
        P.add("dve", lambda e, nh=nh: e.tensor_copy(out=gfin[:, sl(nh, 512)], in_=bk[1 + nh][:, 0:512]), reads=[("bk", 1 + nh)], writes=["gfin"])
    bank_ctr[0] = 3

    wg = {}

    def defgroup(name, nk, width, srcs):
        scr = nc.dram_tensor(f"scr_{name}", [128, nk, width], BF16, kind="Internal").ap()
        wg[name] = dict(nk=nk, width=width, srcs=srcs, scr=scr, conv=False)

    w_in_v = w_in.rearrange("(kc p) n -> p kc n", p=128)
    in_cols = [("in0", 0, 512), ("in1", 512, 512), ("in2", 1024, 512), ("in3", 1536, 16),
               ("in4", 1552, 512), ("in5", 2064, 512), ("in6", 2576, 512)]
    for name, c0, w in in_cols:
        defgroup(name, 8, w, [(0, w, w_in_v[:, :, c0:c0 + w])])
    w_out_v = w_out.rearrange("(kc p) n -> p kc n", p=128)
    w_pg_v = w_ple_gate.rearrange("(kc p) n -> p kc n", p=128)
    w_pp_v = w_ple_proj.rearrange("(kc p) n -> p kc n", p=128)
    for nh in range(2):
        defgroup(f"out{nh}", 8, 512, [(0, 512, w_out_v[:, :, sl(nh, 512)])])
        defgroup(f"pg{nh}", 8, 512, [(0, 512, w_pg_v[:, :, sl(nh, 512)])])
        defgroup(f"pp{nh}", 2, 512, [(0, 512, w_pp_v[:, :, sl(nh, 512)])])
    w_up_v = w_up.rearrange("(kc p) n -> p kc n", p=128)
    for g in range(11):
        defgroup(f"up{g}", 8, 512, [(0, 256, w_up_v[:, :, 256 * g:256 * g + 256]),
                                    (256, 256, w_up_v[:, :, DFF + 256 * g:DFF + 256 * g + 256])])
    w_dn_v = w_down.rearrange("(j p) n -> p j n", p=128)
    DN_GROUPS = [(0, 8), (8, 16), (16, 22)]
    for nh in range(2):
        for jg, (j0, j1) in enumerate(DN_GROUPS):
            defgroup(f"dn{nh}_{jg}", j1 - j0, 512, [(0, 512, w_dn_v[:, j0:j1, sl(nh, 512)])])

    ws_ctr = [0]
    st_ctr = [0]

    def wst_ids(st):
        return [("wst", st)] + [("act", j) for j in range(4 * st, 4 * st + 4)]

    def stage_and_cast(g, dst_tile, dst_ids):
        nk, width = g["nk"], g["width"]
        for k0 in range(0, nk, 4):
            k1 = min(nk, k0 + 4)
            st = st_ctr[0] % 2
            st_ctr[0] += 1
            for (dc, w, src) in g["srcs"]:
                P.add("sp", lambda e, st=st, k0=k0, k1=k1, dc=dc, w=w, src=src: e.dma_start(out=wstage[st][:, 0:k1 - k0, dc:dc + w], in_=src[:, k0:k1, :]),
                      writes=wst_ids(st), dsem=f"d_st{st}")
            P.add("pool", lambda e, st=st, k0=k0, k1=k1, width=width: e.tensor_copy(out=dst_tile[:, k0:k1, 0:width], in_=wstage[st][:, 0:k1 - k0, 0:width]),
                  reads=wst_ids(st), writes=dst_ids)

    def to_scratch(name, src_tile, src_ids):
        g = wg[name]
        nk, width = g["nk"], g["width"]
        P.add("pool", lambda e, nk=nk, width=width, scr=g["scr"]: e.dma_start(out=scr, in_=src_tile[:, 0:nk, 0:width]),
              reads=src_ids, writes=[("scr", name)], dsem=f"d_scr_{name}")
        g["conv"] = True

    held = set()

    def rel_w(*slots):
        for sl_ in slots:
            held.discard(sl_)

    def get_w(name):
        g = wg[name]
        while True:
            slot = ws_ctr[0] % NWS
            ws_ctr[0] += 1
            if slot not in held:
                break
        held.add(slot)
        nk, width = g["nk"], g["width"]
        while not g["conv"]:
            bg_step()
        assert g["conv"], name
        if True:
            P.add("sp", lambda e, slot=slot, nk=nk, width=width, scr=g["scr"]: e.dma_start(out=wslot[slot][:, 0:nk, 0:width], in_=scr),
                  reads=[("scr", name, k0) for k0 in range(0, nk, 2)], writes=[("ws", slot)], dsem=f"d_ws{slot}")
        return slot

    BG_ORDER = (["in1", "in3", "in0", "in2", "in5", "in6", "in4", "out0", "out1"] + [f"up{g}" for g in range(11)]
                + [f"dn{nh}_{jg}" for nh in range(2) for jg in range(3)] + ["pg0", "pp0", "pg1", "pp1"])
    NST = 5
    LOOK = 4

    def conv_gen():
        items = []
        for name in BG_ORDER:
            g = wg[name]
            for k0 in range(0, g["nk"], 2):
                items.append((name, k0, min(g["nk"], k0 + 2)))

        def load(i):
            name, k0, k1 = items[i]
            st = i % NST
            for (dc, w, src) in wg[name]["srcs"]:
                P.add("sp", lambda e, st=st, k0=k0, k1=k1, dc=dc, w=w, src=src: e.dma_start(out=wstage[st][:, 0:k1 - k0, dc:dc + w], in_=src[:, k0:k1, :]),
                      writes=wst_ids(st), dsem=f"d_st{st}")

        for i in range(min(LOOK, len(items))):
            load(i)
        for i, (name, k0, k1) in enumerate(items):
            st = i % NST
            q = i % 4
            width = wg[name]["width"]
            mids = [("mixT", 2 * q), ("mixT", 2 * q + 1)]
            P.add("pool", lambda e, st=st, k0=k0, k1=k1, q=q, width=width: e.tensor_copy(out=mixT[:, 2 * q:2 * q + k1 - k0, 0:width], in_=wstage[st][:, 0:k1 - k0, 0:width]),
                  reads=wst_ids(st), writes=mids)
            P.add("sp", lambda e, k0=k0, k1=k1, q=q, width=width, scr=wg[name]["scr"]: e.dma_start(out=scr[:, k0:k1, :], in_=mixT[:, 2 * q:2 * q + k1 - k0, 0:width]),
                  reads=mids, writes=[("scr", name, k0)], dsem=f"d_sw{q}")
            if i + LOOK < len(items):
                load(i + LOOK)
            if k1 == wg[name]["nk"]:
                wg[name]["conv"] = True
                yield "c"

    def cv(col):
        return cvec[:, col:col + 1]

    def rms_stats(hb, NS):
        h, ss, rs = hT[hb], ssT[hb], rsT[hb]
        for s in range(NS):
            P.add("act", lambda e, s=s: e.activation(out=junk[:], in_=h[:, s, :], func=AF.Square, accum_out=ss[:, s:s + 1]),
                  reads=[("h", hb, s)], writes=["junk", ("ss", hb, s)])
        P.add("dve", lambda e: e.tensor_scalar(out=rs[:, 0:NS], in0=ss[:, 0:NS], scalar1=1.0 / D, scalar2=EPS, op0=ALU.mult, op1=ALU.add),
              reads=[("ss", hb, s) for s in range(NS)], writes=[("rs", hb)])
        P.add("act", lambda e: e.activation(out=rs[:, 0:NS], in_=rs[:, 0:NS], func=AF.Sqrt), reads=[("rs", hb)], writes=[("rs", hb)])
        P.add("dve", lambda e: e.reciprocal(out=rs[:, 0:NS], in_=rs[:, 0:NS]), reads=[("rs", hb)], writes=[("rs", hb)])

    def norm_T(hb, NS, gcol):
        h, rs, yT, ybf = hT[hb], rsT[hb], yTT[hb], ybfT[hb]
        rms_stats(hb, NS)

        def scale(s):
            yb = s % 2
            P.add("act", lambda e, s=s, yb=yb: e.activation(out=ybf[yb][:], in_=h[:, s, :], func=AF.Copy, scale=rs[:, s:s + 1]),
                  reads=[("h", hb, s), ("rs", hb)], writes=[("ybf", hb, yb)])

        scale(0)
        yield "n"
        for s in range(NS):
            yb = s % 2
            b = nb()
            for kc in range(KC):
                P.add("pe", lambda e, b=b, kc=kc, yb=yb: e.transpose(out=bkb[b][:, sl(kc)], in_=ybf[yb][:, sl(kc)], identity=ident[:]),
                      reads=[("ybf", hb, yb), "ident"], writes=[("bk", b)])
            if s + 1 < NS:
                scale(s + 1)
            for kc in range(KC):
                if s % 2 == 0:
                    P.add("dve", lambda e, b=b, kc=kc, s=s: e.tensor_scalar(out=yT[:, kc, sl(s)], in0=bkb[b][:, sl(kc)], scalar1=cv(gcol + kc), scalar2=None, op0=ALU.mult),
                          reads=[("bk", b), "cvec"], writes=[("yT", hb, s)])
                else:
                    P.add("act", lambda e, b=b, kc=kc, s=s: e.activation(out=yT[:, kc, sl(s)], in_=bkb[b][:, sl(kc)], func=AF.Copy, scale=cv(gcol + kc)),
                          reads=[("bk", b), "cvec"], writes=[("yT", hb, s)])
            yield "n"

    def yT_ids(hb, NS):
        return [("yT", hb, s) for s in range(NS)]

    def proj_fm(hb, ws, c0, Tt, NS):
        yT = yTT[hb]
        b = nb()
        for kc in range(KC):
            P.add("pe", lambda e, b=b, kc=kc: e.matmul(bk[b][:, 0:Tt], lhsT=wslot[ws][:, kc, c0:c0 + 128], rhs=yT[:, kc, 0:Tt], start=(kc == 0), stop=(kc == KC - 1)),
                  reads=[("ws", ws)] + yT_ids(hb, NS), writes=[("bk", b)])
        return b

    def resid_add(hb, b, s, nh):
        h = hT[hb]
        P.add("dve", lambda e: e.tensor_tensor(out=h[:, s, sl(nh, 512)], in0=h[:, s, sl(nh, 512)], in1=bk[b][:, 0:512], op=ALU.add),
              reads=[("h", hb, s), ("bk", b)], writes=[("h", hb, s)])

    def tile(mode, src, r0, NS, hb, out_r0=None):
        Tt = NS * 128
        full = mode == "full"
        light = mode == "light"
        h, yT, rs = hT[hb], yTT[hb], rsT[hb]
        P.add("sp", lambda e: e.dma_start(out=h[:, 0:NS, :], in_=src[r0:r0 + Tt, :].rearrange("(s p) d -> p s d", p=128)),
              writes=[("h", hb, s) for s in range(NS)], dsem=f"d_x{hb}")
        yield from norm_T(hb, NS, C_ATTN)
        yield "A"
        ws = get_w("in1")
        for s in range(NS):
            b = nb()
            for kc in range(KC):
                P.add("pe", lambda e, b=b, kc=kc, s=s, ws=ws: e.matmul(bk[b][:, 0:512], lhsT=yT[:, kc, sl(s)], rhs=wslot[ws][:, kc, 0:512], start=(kc == 0), stop=(kc == KC - 1)),
                      reads=[("ws", ws), ("yT", hb, s)], writes=[("bk", b)])
            P.add("act", lambda e, b=b, s=s: e.activation(out=vtok[:, s, :], in_=bk[b][:, 0:512], func=AF.Copy),
                  reads=[("bk", b)], writes=[("vtok", s)])
            yield "c"
        rel_w(ws)
        if not light:
            ws = get_w("in2")
            for c in range(4):
                b = proj_fm(hb, ws, c * 128, Tt, NS)
                P.add("act", lambda e, b=b, c=c: e.activation(out=sg[:, c, 0:Tt], in_=bk[b][:, 0:Tt], func=AF.Silu),
                      reads=[("bk", b)], writes=[("sg", c)])
                yield "c"
        if not light:
            rel_w(ws)
        ws = get_w("in3")
        b = nb()
        for kc in range(KC):
            P.add("pe", lambda e, b=b, kc=kc, ws=ws: e.matmul(bk[b][0:16, 0:Tt], lhsT=wslot[ws][:, kc, 0:16], rhs=yT[:, kc, 0:Tt], start=(kc == 0), stop=(kc == KC - 1)),
                  reads=[("ws", ws)] + yT_ids(hb, NS), writes=[("bk", b)])
        P.add("dve", lambda e, b=b: e.tensor_copy(out=aaug[0:16, 0:Tt], in_=bk[b][0:16, 0:Tt]), reads=[("bk", b)], writes=["aaug"])
        rel_w(ws)
        yield "c"
        for s in range(NS):
            b = nb()
            P.add("pe", lambda e, b=b, s=s: e.matmul(bk[b][:, 0:256], lhsT=aaug[0:17, sl(s)], rhs=walpha[0:17, :], start=True, stop=True),
                  reads=["aaug", "walpha"], writes=[("bk", b)])
            xi = s % 2
            P.add("act", lambda e, b=b, xi=xi: e.activation(out=ex[xi][:], in_=bk[b][:, 0:256], func=AF.Exp, scale=-1.0),
                  reads=[("bk", b)], writes=[("ex", xi)])
            P.add("act", lambda e, s=s, xi=xi: e.activation(out=Lt[:, s, :], in_=ex[xi][:], func=AF.Ln, bias=one1[:, 0:1]),
                  reads=[("ex", xi), "one1"], writes=[("Lt", s)])
            if s % 2 == 1:
                yield "c"
        yield "c"
        for c in range(2):
            b = nb()
            for s in range(NS):
                P.add("pe", lambda e, b=b, s=s, c=c: e.matmul(bk[b][:, sl(s)], lhsT=Lt[:, s, sl(c)], rhs=trineg[:], start=True, stop=True),
                      reads=[("Lt", s), "trineg"], writes=[("bk", b)])
            P.add("act", lambda e, b=b, c=c: e.activation(out=eb[:, c, 0:Tt], in_=bk[b][:, 0:Tt], func=AF.Exp),
                  reads=[("bk", b)], writes=[("eb", c)])
            P.add("act", lambda e, b=b, c=c: e.activation(out=enb[:, c, 0:Tt], in_=bk[b][:, 0:Tt], func=AF.Exp, scale=-1.0),
                  reads=[("bk", b)], writes=[("enb", c)])
            yield "c"
        ws = get_w("in0")
        for c in range(2):
            if not light:
                b = proj_fm(hb, ws, c * 128, Tt, NS)
                for hh in range(2):
                    P.add("dve", lambda e, b=b, c=c, hh=hh: e.scalar_tensor_tensor(out=qdz[hh][:, c, 0:Tt], in0=bk[b][:, 0:Tt], scalar=hm[:, hh:hh + 1], in1=eb[:, c, 0:Tt], op0=ALU.mult, op1=ALU.mult),
                          reads=[("bk", b), ("eb", c), "hm"], writes=[("qd", c, hh)])
                yield "c"
            b = proj_fm(hb, ws, 256 + c * 128, Tt, NS)
            P.add("dve", lambda e, b=b, c=c: e.tensor_tensor(out=kd[:, c, 0:Tt], in0=bk[b][:, 0:Tt], in1=enb[:, c, 0:Tt], op=ALU.mult),
                  reads=[("bk", b), ("enb", c)], writes=[("kd", c)])
            yield "c"
        rel_w(ws)
        for s in range(NS):
            b = nb()
            for c in range(2):
                P.add("pe", lambda e, b=b, c=c, s=s: e.transpose(out=bkb[b][:, sl(c)], in_=kd[:, c, sl(s)], identity=ident[:]),
                      reads=[("kd", c), "ident"], writes=[("bk", b)])
            P.add("dve", lambda e, b=b, s=s: e.tensor_copy(out=kdtok[s][:], in_=bkb[b][:, 0:256]), reads=[("bk", b)], writes=[("kdtok", s)])
            if not light:
                b2 = nb()
                for hh4 in range(4):
                    c, hh = divmod(hh4, 2)
                    P.add("pe", lambda e, b2=b2, hh4=hh4, c=c, hh=hh, s=s: e.matmul(bk[b2][:, sl(hh4)], lhsT=kd[:, c, sl(s)], rhs=qdz[hh][:, c, sl(s)], start=True, stop=True),
                          reads=[("kd", c), ("qd", c, hh)], writes=[("bk", b2)])
                P.add("dve", lambda e, b2=b2, s=s: e.tensor_tensor(out=scm[s][:], in0=bk[b2][:, 0:512], in1=mtri4[:], op=ALU.mult),
                      reads=[("bk", b2), "mtri4"], writes=[("scm", s)])
            yield "c"
        for s in range(NS):
            if not light:
                b3 = nb()
                for hh4 in range(4):
                    c, hh = divmod(hh4, 2)
                    P.add("pe", lambda e, b3=b3, hh4=hh4, s=s: e.matmul(bk[b3][:, sl(hh4)], lhsT=vtok[:, s, sl(hh4)], rhs=scm[s][:, sl(hh4)], start=True, stop=False),
                          reads=[("vtok", s), ("scm", s)], writes=[("bk", b3)])
                    P.add("pe", lambda e, b3=b3, hh4=hh4, c=c, hh=hh, s=s: e.matmul(bk[b3][:, sl(hh4)], lhsT=Sbf[:, c, sl(hh)], rhs=qdz[hh][:, c, sl(s)], start=False, stop=True),
                          reads=[("Sbf", c), ("qd", c, hh)], writes=[("bk", b3)])
                P.add("act", lambda e, b3=b3, s=s: e.activation(out=oT[:, :, sl(s)], in_=bk[b3][:, 0:512].rearrange("p (a i) -> p a i", a=4), func=AF.Copy),
                      reads=[("bk", b3)], writes=[("oT", a) for a in range(4)])
                yield "c"
            for c in range(2):
                b4 = nb()
                P.add("pe", lambda e, b4=b4, c=c, s=s: e.matmul(bk[b4][:, 0:256], lhsT=kdtok[s][:, sl(c)], rhs=vtok[:, s, sl(c, 256)], start=True, stop=True),
                      reads=[("kdtok", s), ("vtok", s)], writes=[("bk", b4)])
                P.add("dve", lambda e, b4=b4, c=c: e.scalar_tensor_tensor(out=R[:, c, :], in0=R[:, c, :], scalar=dsv[:, c:c + 1], in1=bk[b4][:, 0:256], op0=ALU.mult, op1=ALU.add),
                      reads=[("R", c), ("dsv", c), ("bk", b4)], writes=[("R", c)])
                col = s * 128 + 127
                P.add("dve", lambda e, c=c, col=col: e.tensor_copy(out=dsv[:, c:c + 1], in_=eb[:, c, col:col + 1]),
                      reads=[("eb", c)], writes=[("dsv", c)])
                P.add("act", lambda e, c=c, col=col: e.activation(out=Sbf[:, c, :], in_=R[:, c, :], func=AF.Copy, scale=eb[:, c, col:col + 1]),
                      reads=[("R", c), ("eb", c)], writes=[("Sbf", c)])
            yield "c"
        if light:
            yield "M"
            return
        yield "H"
        for a in range(4):
            P.add("act", lambda e, a=a: e.activation(out=osq[:, a, 0:Tt], in_=oT[:, a, 0:Tt], func=AF.Square),
                  reads=[("oT", a)], writes=[("osq", a)])
        yield "c"
        hn = []
        for a in range(4):
            b = nb()
            P.add("pe", lambda e, b=b, a=a: e.matmul(bk[b][:, 0:Tt], lhsT=onesdv[:], rhs=osq[:, a, 0:Tt], start=True, stop=True),
                  reads=["onesdv", ("osq", a)], writes=[("bk", b)])
            t1 = nt()
            P.add("act", lambda e, b=b, t1=t1: e.activation(out=tmp[t1][:, 0:Tt], in_=bk[b][:, 0:Tt], func=AF.Sqrt, bias=epsT[:, 0:1]),
                  reads=[("bk", b), "epsT"], writes=[("tmp", t1)])
            P.add("dve", lambda e, t1=t1: e.reciprocal(out=tmp[t1][:, 0:Tt], in_=tmp[t1][:, 0:Tt]), reads=[("tmp", t1)], writes=[("tmp", t1)])
            P.add("dve", lambda e, t1=t1, a=a: e.tensor_tensor(out=tmp[t1][:, 0:Tt], in0=oT[:, a, 0:Tt], in1=tmp[t1][:, 0:Tt], op=ALU.mult),
                  reads=[("tmp", t1), ("oT", a)], writes=[("tmp", t1)])
            P.add("dve", lambda e, t1=t1, a=a: e.scalar_tensor_tensor(out=mixT[:, a, 0:Tt], in0=tmp[t1][:, 0:Tt], scalar=cv(C_HG + a), in1=sg[:, a, 0:Tt], op0=ALU.mult, op1=ALU.mult),
                  reads=[("tmp", t1), "cvec", ("sg", a)], writes=[("mixT", a)])
            yield "c"
        if not light:
            ws = get_w("in5")
            for c in range(4):
                b = proj_fm(hb, ws, c * 128, Tt, NS)
                P.add("act", lambda e, b=b, c=c: e.activation(out=oT[:, c, 0:Tt], in_=bk[b][:, 0:Tt], func=AF.Copy),
                      reads=[("bk", b)], writes=[("oT", c)])
                yield "c"
            rel_w(ws)
            ws = get_w("in6")
            for c in range(4):
                b = proj_fm(hb, ws, c * 128, Tt, NS)
                P.add("dve", lambda e, b=b, c=c: e.tensor_tensor(out=ubuf[:, c, 2:2 + Tt], in0=bk[b][:, 0:Tt], in1=oT[:, c, 0:Tt], op=ALU.mult),
                      reads=[("bk", b), ("oT", c)], writes=[("ub", c)])
                yield "c"
            rel_w(ws)
            ws = get_w("in4")
            for c in range(4):
                b = proj_fm(hb, ws, c * 128, Tt, NS)
                t1 = nt()
                P.add("act", lambda e, c=c, t1=t1: e.activation(out=tmp[t1][:, 0:Tt], in_=ubuf[:, c, 0:Tt], func=AF.Copy, scale=cv(C_MCW + 0 * 4 + c)),
                      reads=[("ub", c), "ubh", "cvec"], writes=[("tmp", t1)])
                P.add("dve", lambda e, c=c, t1=t1: e.scalar_tensor_tensor(out=tmp[t1][:, 0:Tt], in0=ubuf[:, c, 1:1 + Tt], scalar=cv(C_MCW + 1 * 4 + c), in1=tmp[t1][:, 0:Tt], op0=ALU.mult, op1=ALU.add),
                      reads=[("ub", c), "ubh", "cvec", ("tmp", t1)], writes=[("tmp", t1)])
                P.add("dve", lambda e, c=c, t1=t1: e.scalar_tensor_tensor(out=tmp[t1][:, 0:Tt], in0=ubuf[:, c, 2:2 + Tt], scalar=cv(C_MCW + 2 * 4 + c), in1=tmp[t1][:, 0:Tt], op0=ALU.mult, op1=ALU.add),
                      reads=[("ub", c), "cvec", ("tmp", t1)], writes=[("tmp", t1)])
                P.add("dve", lambda e, c=c, t1=t1, b=b: e.tensor_tensor(out=mixT[:, 4 + c, 0:Tt], in0=tmp[t1][:, 0:Tt], in1=bk[b][:, 0:Tt], op=ALU.mult),
                      reads=[("tmp", t1), ("bk", b)], writes=[("mixT", 4 + c)])
                yield "c"
            rel_w(ws)
            P.add("pool", lambda e: e.tensor_copy(out=ubuf[:, :, 0:2], in_=ubuf[:, :, Tt:Tt + 2]),
                  reads=[("ub", c) for c in range(4)], writes=["ubh"])
        yield "M"
        for nh in range(2):
            ws = get_w(f"out{nh}")
            for s in range(NS):
                b = nb()
                for kc in range(KC):
                    P.add("pe", lambda e, b=b, kc=kc, s=s, ws=ws: e.matmul(bk[b][:, 0:512], lhsT=mixT[:, kc, sl(s)], rhs=wslot[ws][:, kc, 0:512], start=(kc == 0), stop=(kc == KC - 1)),
                          reads=[("ws", ws), ("mixT", kc)], writes=[("bk", b)])
                resid_add(hb, b, s, nh)
                yield "c"
            rel_w(ws)
        yield "W"
        if full:
            P.add("sp", lambda e: e.dma_start(out=pbuf[:, 0:NS, :], in_=p_d[r0:r0 + Tt, :].rearrange("(s p) d -> p s d", p=128)),
                  writes=["pbuf"], dsem="d_p")
            P.add("pool", lambda e: e.tensor_copy(out=pbf[:, 0:NS, :], in_=pbuf[:, 0:NS, :]), reads=["pbuf"], writes=["pbf"])
        yield from norm_T(hb, NS, C_FFN)
        ws = None
        for g in range(11):
            if ws is not None:
                rel_w(ws)
            ws = get_w(f"up{g}")
            for jj in range(2):
                j = 2 * g + jj
                bg = proj_fm(hb, ws, jj * 128, Tt, NS)
                gi = j % 3
                P.add("pool", lambda e, gi=gi, j=j: e.tensor_copy(out=gbuf[gi][:, 0:2], in_=ghalo[:, j, :]),
                      reads=[("gh", j)], writes=[("gbh", gi)])
                P.add("act", lambda e, gi=gi, bg=bg: e.activation(out=gbuf[gi][:, 2:2 + Tt], in_=bk[bg][:, 0:Tt], func=AF.Copy),
                      reads=[("bk", bg)], writes=[("gb", gi)])
                P.add("pool", lambda e, gi=gi, j=j: e.tensor_copy(out=ghalo[:, j, :], in_=gbuf[gi][:, Tt:Tt + 2]),
                      reads=[("gb", gi)], writes=[("gh", j)])
                if not full:
                    yield "c"
                    continue
                bu = proj_fm(hb, ws, 256 + jj * 128, Tt, NS)
                t1 = nt()
                P.add("act", lambda e, gi=gi, j=j, t1=t1: e.activation(out=tmp[t1][:, 0:Tt], in_=gbuf[gi][:, 0:Tt], func=AF.Copy, scale=cv(C_FCW + 0 * NJ + j)),
                      reads=[("gb", gi), ("gbh", gi), "cvec"], writes=[("tmp", t1)])
                P.add("dve", lambda e, gi=gi, j=j, t1=t1: e.scalar_tensor_tensor(out=tmp[t1][:, 0:Tt], in0=gbuf[gi][:, 1:1 + Tt], scalar=cv(C_FCW + 1 * NJ + j), in1=tmp[t1][:, 0:Tt], op0=ALU.mult, op1=ALU.add),
                      reads=[("gb", gi), ("gbh", gi), "cvec", ("tmp", t1)], writes=[("tmp", t1)])
                P.add("dve", lambda e, gi=gi, j=j, t1=t1: e.scalar_tensor_tensor(out=tmp[t1][:, 0:Tt], in0=gbuf[gi][:, 2:2 + Tt], scalar=cv(C_FCW + 2 * NJ + j), in1=tmp[t1][:, 0:Tt], op0=ALU.mult, op1=ALU.add),
                      reads=[("gb", gi), "cvec", ("tmp", t1)], writes=[("tmp", t1)])
                P.add("act", lambda e, t1=t1: e.activation(out=tmp[t1][:, 0:Tt], in_=tmp[t1][:, 0:Tt], func=AF.Silu),
                      reads=[("tmp", t1)], writes=[("tmp", t1)])
                P.add("dve", lambda e, t1=t1, j=j, bu=bu: e.tensor_tensor(out=actT[:, j, 0:Tt], in0=tmp[t1][:, 0:Tt], in1=bk[bu][:, 0:Tt], op=ALU.mult),
                      reads=[("tmp", t1), ("bk", bu)], writes=[("act", j)])
                yield "c"
        rel_w(ws)
        if not full:
            return
        for nh in range(2):
            banks4 = reserve(NS)
            for jg, (j0, j1) in enumerate(DN_GROUPS):
                ws = get_w(f"dn{nh}_{jg}")
                for s in range(NS):
                    for j in range(j0, j1):
                        P.add("pe", lambda e, b=banks4[s], j=j, j0=j0, s=s, ws=ws: e.matmul(bk[b][:, 0:512], lhsT=actT[:, j, sl(s)], rhs=wslot[ws][:, j - j0, 0:512], start=(j == 0), stop=(j == NJ - 1)),
                              reads=[("ws", ws), ("act", j)], writes=[("bk", banks4[s])])
                    yield "c"
                rel_w(ws)
            for s in range(NS):
                resid_add(hb, banks4[s], s, nh)
            release(banks4)
            yield "c"
        yield from norm_T(hb, NS, C_PLE)
        for s in range(NS):
            b = nb()
            for kc in range(2):
                P.add("pe", lambda e, b=b, kc=kc, s=s: e.transpose(out=bkb[b][:, sl(kc)], in_=pbf[:, s, sl(kc)], identity=ident[:]),
                      reads=["pbf", "ident"], writes=[("bk", b)])
            P.add("dve", lambda e, b=b, s=s: e.tensor_copy(out=pT[:, :, sl(s)], in_=bkb[b][:, 0:256].rearrange("p (a i) -> p a i", a=2)),
                  reads=[("bk", b)], writes=[("pT", s)])
        yield "c"
        for nh in range(2):
            wsg = get_w(f"pg{nh}")
            wsp = get_w(f"pp{nh}")
            for s in range(NS):
                bg = nb()
                for kc in range(KC):
                    P.add("pe", lambda e, bg=bg, kc=kc, s=s, wsg=wsg: e.matmul(bk[bg][:, 0:512], lhsT=yT[:, kc, sl(s)], rhs=wslot[wsg][:, kc, 0:512], start=(kc == 0), stop=(kc == KC - 1)),
                          reads=[("ws", wsg), ("yT", hb, s)], writes=[("bk", bg)])
                bp = nb()
                for kc in range(2):
                    P.add("pe", lambda e, bp=bp, kc=kc, s=s, wsp=wsp: e.matmul(bk[bp][:, 0:512], lhsT=pT[:, kc, sl(s)], rhs=wslot[wsp][:, kc, 0:512], start=(kc == 0), stop=(kc == 1)),
                          reads=[("ws", wsp), ("pT", s)], writes=[("bk", bp)])
                t1 = nt()
                P.add("act", lambda e, t1=t1, bg=bg: e.activation(out=tmp[t1][:], in_=bk[bg][:, 0:512], func=AF.Sigmoid),
                      reads=[("bk", bg)], writes=[("tmp", t1)])
                P.add("dve", lambda e, t1=t1, bp=bp: e.tensor_tensor(out=tmp[t1][:], in0=tmp[t1][:], in1=bk[bp][:, 0:512], op=ALU.mult),
                      reads=[("tmp", t1), ("bk", bp)], writes=[("tmp", t1)])
                P.add("pool", lambda e, t1=t1, s=s, nh=nh: e.tensor_tensor(out=h[:, s, sl(nh, 512)], in0=h[:, s, sl(nh, 512)], in1=tmp[t1][:], op=ALU.add),
                      reads=[("tmp", t1), ("h", hb, s)], writes=[("h", hb, s)])
                yield "c"
            rel_w(wsg, wsp)
        rms_stats(hb, NS)
        yield "c"
        for s in range(NS):
            P.add("dve", lambda e, s=s: e.scalar_tensor_tensor(out=h[:, s, :], in0=h[:, s, :], scalar=rs[:, s:s + 1], in1=gfin[:], op0=ALU.mult, op1=ALU.mult),
                  reads=[("h", hb, s), ("rs", hb), "gfin"], writes=[("h", hb, s)])
            P.add("pool", lambda e, s=s: e.dma_start(out=out_d[out_r0 + s * 128:out_r0 + (s + 1) * 128, :], in_=h[:, s, :]),
                  reads=[("h", hb, s)], writes=[("o", out_r0, s)], dsem=f"d_o{hb}")
            yield "c"

    specs = []
    r = 0
    nl = NL
    while nl > 0:
        ns = min(4, nl)
        specs.append(("light", xp_d, r, ns, None))
        r += ns * 128
        nl -= ns
    if HALO:
        specs.append(("halo", xp_d, r, 1, None))
    out_ids = []
    for t in range(NF):
        specs.append(("full", x_d, t * 512, 4, t * 512))
        out_ids += [("o", t * 512, s) for s in range(4)]
    runs = []
    for i, (mode, src, r0, ns, o0) in enumerate(specs):
        runs.append(dict(g=tile(mode, src, r0, ns, i % 2, out_r0=o0), tag=None, fin=False, mode=mode))
    bg = dict(g=conv_gen(), fin=False)

    def bg_step():
        if not bg["fin"]:
            try:
                next(bg["g"])
            except StopIteration:
                bg["fin"] = True

    def one(t):
        try:
            t["tag"] = next(t["g"])
        except StopIteration:
            t["fin"] = True
            t["tag"] = "END"

    def reached(t, targets):
        return t is None or t["fin"] or t["tag"] in targets

    nstep = [0]

    def interleave(a, ta, b, tb, ra=2, rb=1):
        while not (reached(a, ta) and reached(b, tb)):
            for _ in range(ra):
                if reached(a, ta):
                    break
                one(a)
            for _ in range(rb):
                if reached(b, tb):
                    break
                one(b)
            nstep[0] += 1
            if nstep[0] % 2 == 0:
                bg_step()

    def drain_bg():
        while not bg["fin"]:
            bg_step()

    if runs[0]["mode"] != "light":
        drain_bg()
    while not wg["in1"]["conv"]:
        bg_step()
    for i, cur in enumerate(runs):
        nxt = runs[i + 1] if i + 1 < len(runs) else None
        if nxt is not None and nxt["mode"] != "light":
            drain_bg()
        if cur["mode"] == "light":
            interleave(cur, {"W"}, nxt, {"A"})
        else:
            interleave(cur, {"W"}, None, {"A"})
        credit = 0.0
        while not reached(cur, {"END"}):
            one(cur)
            credit += 2.0 if cur["tag"] == "n" else 0.75
            while credit >= 1.0 and not reached(nxt, {"M"}):
                one(nxt)
                credit -= 1.0
            if reached(nxt, {"M"}):
                credit = 0.0
            nstep[0] += 1
            if nstep[0] % 2 == 0:
                bg_step()
    while not bg["fin"]:
        bg_step()
    P.add("sp", None, reads=out_ids)

    stats = P.analyze()
    keys = P.sem_keys()
    sems = {k: es.enter_context(nc.semaphore(f"s{i}")) for i, k in enumerate(keys)}
    with nc.Block() as block:
        P.emit(block, sems)
    es.close()
    return nc, stats


_NAMES = ["attn_norm", "w_in", "w_alpha_up", "b_alpha", "gla_head_norm", "mix_conv_w", "w_out", "ffn_norm",
          "w_up", "ffn_conv_w", "w_down", "ple_norm", "w_ple_gate", "w_ple_proj"]


def make_in_maps(inputs, n_cores=8):
    x = np.asarray(inputs["x"], dtype=np.float32)
    p = np.asarray(inputs["p"], dtype=np.float32)
    common = {k: np.ascontiguousarray(np.asarray(inputs[k], dtype=np.float32)[0]) for k in _NAMES}
    common["final_norm"] = np.ascontiguousarray(np.asarray(inputs["final_norm"], dtype=np.float32))
    maps = []
    for c in range(n_cores):
        b, hf = divmod(c, 2)
        m = dict(common)
        m["x"] = np.ascontiguousarray(x[b, hf * SEQ_HALF:(hf + 1) * SEQ_HALF])
        m["p"] = np.ascontiguousarray(p[0, b, hf * SEQ_HALF:(hf + 1) * SEQ_HALF])
        if hf == 1:
            m["xp"] = np.ascontiguousarray(x[b, 0:SEQ_HALF])
        else:
            m["xp"] = np.zeros((SEQ_HALF, D), np.float32)
        maps.append(m)
    return maps


def kernel(**inputs):
    nc, _ = build_nc()
    maps = make_in_maps(inputs)
    res = run_bass_kernel_spmd(nc, maps, core_ids=list(range(8)))
    x = inputs["x"]
    out = np.empty((4, 2 * SEQ_HALF, D), np.float32)
    for c in range(8):
        b, hf = divmod(c, 2)
        out[b, hf * SEQ_HALF:(hf + 1) * SEQ_HALF] = np.asarray(res.results[c]["out"], dtype=np.float32)
    return out
```

```python
import numpy as np
from contextlib import ExitStack
import concourse.bass as bass
import concourse.mybir as mybir
from concourse.bass_utils import run_bass_kernel_spmd

F32 = mybir.dt.float32
BF16 = mybir.dt.bfloat16
AF = mybir.ActivationFunctionType
ALU = mybir.AluOpType

ENGS = ("pe", "act", "dve", "pool", "sp")
SAME_ENG_RAW_WINDOW = 4
STRICT_SAME_ENGINE = True

D = 1024
KC = 8
DIN = 3088
DFF = 2816
NJ = 22
PLE = 256
EPS = 1e-6
SEQ_HALF = 4096


class Op:
    __slots__ = ("eng", "fn", "reads", "writes", "dsem", "idx", "waits", "inc", "val", "pos")


class Prog:
    def __init__(self):
        self.ops = []

    def add(self, eng, fn, reads=(), writes=(), dsem=None):
        o = Op()
        o.eng = eng
        o.fn = fn
        o.reads = tuple(reads)
        o.writes = tuple(writes)
        o.dsem = dsem
        o.idx = len(self.ops)
        o.waits = {}
        o.inc = False
        o.val = None
        o.pos = None
        self.ops.append(o)
        return o

    def analyze(self):
        last_w = {}
        readers = {}
        pos = {e: 0 for e in ENGS}
        dcount = {}
        deps_of = []
        for o in self.ops:
            o.pos = pos[o.eng]
            pos[o.eng] += 1
            if o.dsem is not None:
                dcount[o.dsem] = dcount.get(o.dsem, 0) + 16
                o.val = dcount[o.dsem]
            deps = {}
            for b in o.reads:
                for w in last_w.get(b, ()):
                    deps[w.idx] = (w, "raw")
            for b in o.writes:
                for w in last_w.get(b, ()):
                    if o.dsem is not None and w.dsem == o.dsem and not readers.get(b):
                        continue
                    if w.idx not in deps:
                        deps[w.idx] = (w, "waw")
                for r in readers.get(b, ()):
                    if r.idx not in deps:
                        deps[r.idx] = (r, "war")
            keep = []
            for w, kind in deps.values():
                if w is o:
                    continue
                if w.dsem is None and o.dsem is None and w.eng == o.eng:
                    if w.eng == "pe":
                        continue
                    if not STRICT_SAME_ENGINE:
                        if kind != "raw":
                            continue
                        if o.pos - w.pos > SAME_ENG_RAW_WINDOW:
                            continue
                keep.append(w)
            best = {}
            rest = []
            for w in keep:
                if w.dsem is None:
                    if w.eng not in best or best[w.eng].pos < w.pos:
                        best[w.eng] = w
                else:
                    rest.append(w)
            keep = rest + list(best.values())
            deps_of.append(keep)
            for b in o.reads:
                readers.setdefault(b, []).append(o)
            for b in o.writes:
                lw = last_w.get(b)
                if o.dsem is not None and lw and lw[0].dsem == o.dsem and not readers.get(b):
                    lw.append(o)
                else:
                    last_w[b] = [o]
                readers[b] = []
        for keep in deps_of:
            for w in keep:
                if w.dsem is None:
                    w.inc = True
        cnt = {e: 0 for e in ENGS}
        for o in self.ops:
            if o.dsem is None and o.inc:
                cnt[o.eng] += 1
                o.val = cnt[o.eng]
        waited = {e: {} for e in ENGS}
        nw = 0
        for o, keep in zip(self.ops, deps_of):
            need = {}
            for w in keep:
                key = w.dsem if w.dsem is not None else ("eng", w.eng)
                need[key] = max(need.get(key, 0), w.val)
            wd = waited[o.eng]
            for key, v in need.items():
                if wd.get(key, 0) >= v:
                    continue
                wd[key] = v
                o.waits[key] = v
                nw += 1
        self.stats = dict(nops=len(self.ops), nwaits=nw, incs=dict(cnt), dma=dict(dcount))
        return self.stats

    def emit(self, block, sems):
        by_eng = {e: [o for o in self.ops if o.eng == e] for e in ENGS}

        def body(ename):
            def run(eng):
                for o in by_eng[ename]:
                    for key, v in o.waits.items():
                        eng.wait_ge(sems[key], v)
                    if o.fn is None:
                        continue
                    ins = o.fn(eng)
                    if o.dsem is not None:
                        ins.then_inc(sems[o.dsem], 16)
                    elif o.inc:
                        ins.then_inc(sems[("eng", ename)], 1)
            return run

        block.tensor(body("pe"))
        block.scalar(body("act"))
        block.vector(body("dve"))
        block.gpsimd(body("pool"))
        block.sync(body("sp"))

    def sem_keys(self):
        keys = [("eng", e) for e in ENGS if e != "sp"]
        seen = set()
        for o in self.ops:
            if o.dsem is not None and o.dsem not in seen:
                seen.add(o.dsem)
                keys.append(o.dsem)
        return keys


def sl(s, n=128):
    return slice(s * n, (s + 1) * n)


def build_nc(NL=31, NF=8, HALO=True):
    NPRE = NL * 128 + (128 if HALO else 0)
    NTOK = NF * 512
    nc = bass.Bass("TRN2", target_bir_lowering=False)

    def din(name, shape):
        return nc.dram_tensor(name, list(shape), F32, kind="ExternalInput").ap()

    xp_d = din("xp", [max(NPRE, 128), D])
    x_d = din("x", [NTOK, D])
    p_d = din("p", [NTOK, PLE])
    attn_norm = din("attn_norm", [D])
    w_in = din("w_in", [D, DIN])
    w_alpha_up = din("w_alpha_up", [16, 256])
    b_alpha = din("b_alpha", [256])
    gla_head_norm = din("gla_head_norm", [4, 128])
    mix_conv_w = din("mix_conv_w", [3, 512])
    w_out = din("w_out", [D, D])
    ffn_norm = din("ffn_norm", [D])
    w_up = din("w_up", [D, 2 * DFF])
    ffn_conv_w = din("ffn_conv_w", [3, DFF])
    w_down = din("w_down", [DFF, D])
    ple_norm = din("ple_norm", [D])
    w_ple_gate = din("w_ple_gate", [D, D])
    w_ple_proj = din("w_ple_proj", [PLE, D])
    final_norm = din("final_norm", [D])
    out_d = nc.dram_tensor("out", [NTOK, D], F32, kind="ExternalOutput").ap()

    P = Prog()
    es = ExitStack()

    def sb(name, shape, dt):
        return es.enter_context(nc.sbuf_tensor(name, list(shape), dt))

    hT = [sb(f"h{i}", [128, 4, D], F32) for i in range(2)]
    ybfT = [[sb(f"ybf{p}_{i}", [128, D], BF16) for i in range(2)] for p in range(2)]
    junk = sb("junk", [128, D], BF16)
    ssT = [sb(f"ss{i}", [128, 4], F32) for i in range(2)]
    rsT = [sb(f"rs{i}", [128, 4], F32) for i in range(2)]
    yTT = [sb(f"yT{i}", [128, KC, 512], BF16) for i in range(2)]
    mixT = sb("mixT", [128, KC, 512], BF16)
    big = sb("big", [128, NJ * 256], F32)
    actT = big[:].bitcast(BF16).rearrange("p (j t) -> p j t", j=NJ)
    vtok = sb("vtok", [128, 4, 512], BF16)
    sg = sb("sg", [128, 4, 512], BF16)
    qdz = [sb(f"qdz{i}", [128, 2, 512], BF16) for i in range(2)]
    hm = sb("hm", [128, 2], F32)
    kd = sb("kd", [128, 2, 512], BF16)
    eb = sb("eb", [128, 2, 512], F32)
    enb = sb("enb", [128, 2, 512], BF16)
    Lt = sb("Lt", [128, 4, 256], BF16)
    ex = [sb(f"ex{i}", [128, 256], F32) for i in range(2)]
    aaug = sb("aaug", [32, 512], BF16)
    kdtok = [sb(f"kdtok{i}", [128, 256], BF16) for i in range(4)]
    scm = [sb(f"scm{i}", [128, 512], BF16) for i in range(4)]
    oT = sb("oT", [128, 4, 512], F32)
    osq = sb("osq", [128, 4, 512], BF16)
    R = sb("R", [128, 2, 256], F32)
    Sbf = sb("Sbf", [128, 2, 256], BF16)
    dsv = sb("dsv", [128, 2], F32)
    ubuf = sb("ubuf", [128, 4, 514], F32)
    NTMP = 4
    tmp = [sb(f"tmp{i}", [128, 512], F32) for i in range(NTMP)]
    gbuf = [sb(f"gbuf{i}", [128, 514], F32) for i in range(3)]
    ghalo = sb("ghalo", [128, NJ, 2], F32)
    pbuf = sb("pbuf", [128, 4, PLE], F32)
    pbf = sb("pbf", [128, 4, PLE], BF16)
    pT = sb("pT", [128, 2, 512], BF16)
    NWS = 4
    wslot = [sb(f"wslot{i}", [128, KC, 512], BF16) for i in range(NWS)]
    wstage = [big[:, i * 1024:(i + 1) * 1024].rearrange("p (a b) -> p a b", a=2) for i in range(5)]
    onesf = sb("onesf", [128, 512], F32)
    ident = sb("ident", [128, 128], BF16)
    identf = sb("identf", [128, 128], F32)
    mtri4 = sb("mtri4", [128, 512], BF16)
    negf = sb("negf", [128, 128], F32)
    trineg = sb("trineg", [128, 128], BF16)
    onesdv = sb("onesdv", [128, 128], BF16)
    epsT = sb("epsT", [128, 1], F32)
    one1 = sb("one1", [128, 1], F32)
    craw = sb("craw", [128, 128], F32)
    cvec = sb("cvec", [128, 128], F32)
    gfin = sb("gfin", [128, D], F32)
    walpha = sb("walpha", [32, 256], BF16)

    bk = [es.enter_context(nc.psum_tensor(f"bk{i}", [128, 512], F32)) for i in range(8)]
    bkb = [b[:].bitcast(BF16) for b in bk]
    bank_ctr = [0]

    reserved = set()

    def nb():
        while True:
            i = bank_ctr[0] % 8
            bank_ctr[0] += 1
            if i not in reserved:
                return i

    def reserve(n):
        got = [nb() for _ in range(n)]
        reserved.update(got)
        return got

    def release(bs):
        reserved.difference_update(bs)

    tmp_ctr = [0]

    def nt():
        i = tmp_ctr[0] % NTMP
        tmp_ctr[0] += 1
        return i

    C_ATTN, C_FFN, C_PLE, C_HG, C_MCW, C_FCW = 0, 8, 16, 24, 28, 40

    P.add("pool", lambda e: e.memset(onesf[:], 1.0), writes=["onesf"])
    P.add("pool", lambda e: e.affine_select(out=ident[:], in_=onesf[:, 0:128], pattern=[[-1, 128]], compare_op=ALU.is_equal, fill=0.0, base=0, channel_multiplier=1), reads=["onesf"], writes=["ident"])
    P.add("pool", lambda e: e.affine_select(out=identf[:], in_=onesf[:, 0:128], pattern=[[-1, 128]], compare_op=ALU.is_equal, fill=0.0, base=0, channel_multiplier=1), reads=["onesf"], writes=["identf"])
    P.add("pool", lambda e: e.affine_select(out=mtri4[:].rearrange("p (h i) -> p h i", h=4), in_=onesf[:].rearrange("p (h i) -> p h i", h=4), pattern=[[0, 4], [1, 128]], compare_op=ALU.is_ge, fill=0.0, base=0, channel_multiplier=-1), reads=["onesf"], writes=["mtri4"])
    P.add("pool", lambda e: e.memset(negf[:], -0.0625), writes=["negf"])
    P.add("pool", lambda e: e.affine_select(out=trineg[:], in_=negf[:], pattern=[[1, 128]], compare_op=ALU.is_ge, fill=0.0, base=0, channel_multiplier=-1), reads=["negf"], writes=["trineg"])
    P.add("pool", lambda e: e.memset(onesdv[:], 1.0 / 128.0), writes=["onesdv"])
    P.add("pool", lambda e: e.memset(epsT[:], EPS), writes=["epsT"])
    P.add("pool", lambda e: e.memset(one1[:], 1.0), writes=["one1"])
    P.add("pool", lambda e: e.memset(aaug[:], 1.0), writes=["aaug"])
    P.add("pool", lambda e: e.memset(hm[:], 0.0), writes=["hm"])
    P.add("pool", lambda e: e.memset(hm[0:64, 0:1], 0.125), writes=["hm"])
    P.add("pool", lambda e: e.memset(hm[64:128, 1:2], 0.125), writes=["hm"])
    P.add("pool", lambda e: e.memset(R[:], 0.0), writes=[("R", 0), ("R", 1)])
    P.add("pool", lambda e: e.memset(Sbf[:], 0.0), writes=[("Sbf", 0), ("Sbf", 1)])
    P.add("pool", lambda e: e.memset(dsv[:], 1.0), writes=[("dsv", 0), ("dsv", 1)])
    P.add("pool", lambda e: e.memset(ubuf[:], 0.0), writes=[("ub", c) for c in range(4)] + ["ubh"])
    P.add("pool", lambda e: e.memset(ghalo[:], 0.0), writes=[("gh", j) for j in range(NJ)])
    P.add("pool", lambda e: e.memset(craw[:], 0.0), writes=["craw"])
    crows = [
        (C_ATTN, 8, attn_norm.rearrange("(c p) -> c p", p=128)),
        (C_FFN, 8, ffn_norm.rearrange("(c p) -> c p", p=128)),
        (C_PLE, 8, ple_norm.rearrange("(c p) -> c p", p=128)),
        (C_HG, 4, gla_head_norm),
        (C_MCW, 12, mix_conv_w.rearrange("k (c p) -> (k c) p", p=128)),
        (C_FCW, 66, ffn_conv_w.rearrange("k (j p) -> (k j) p", p=128)),
    ]
    for r0, n, src in crows:
        P.add("sp", lambda e, r0=r0, n=n, src=src: e.dma_start(out=craw[r0:r0 + n, :], in_=src), reads=[], writes=["craw"], dsem="d_c1")
    P.add("pe", lambda e: e.transpose(out=bk[0][:, 0:128], in_=craw[:, :], identity=identf[:]), reads=["craw", "identf"], writes=[("bk", 0)])
    P.add("dve", lambda e: e.tensor_copy(out=cvec[:], in_=bk[0][:, 0:128]), reads=[("bk", 0)], writes=["cvec"])
    P.add("pool", lambda e: e.memset(tmp[1][0:32, 0:256], 0.0), writes=["wa_st", ("tmp", 1)])
    P.add("sp", lambda e: e.dma_start(out=tmp[1][0:16, 0:256], in_=w_alpha_up), writes=["wa_st", ("tmp", 1)], dsem="d_c2")
    P.add("sp", lambda e: e.dma_start(out=tmp[1][16:17, 0:256], in_=b_alpha.rearrange("(o n) -> o n", o=1)), writes=["wa_st", ("tmp", 1)], dsem="d_c2")
    P.add("pool", lambda e: e.tensor_copy(out=walpha[:], in_=tmp[1][0:32, 0:256]), reads=["wa_st", ("tmp", 1)], writes=["walpha"])
    for nh in range(2):
        P.add("sp", lambda e, nh=nh: e.dma_start(out=tmp[2 * nh][0:1, :], in_=final_norm[sl(nh, 512)].rearrange("(o n) -> o n", o=1)), writes=[("gfrow", nh), ("tmp", 2 * nh)], dsem=f"d_c3{nh}")
    for nh in range(2):
        P.add("pe", lambda e, nh=nh: e.matmul(bk[1 + nh][:, 0:512], lhsT=onesf[0:1, 0:128], rhs=tmp[2 * nh][0:1, :], start=True, stop=True), reads=["onesf", ("gfrow", nh), ("tmp", 2 * nh)], writes=[("bk", 1 + nh)])
        P.add("dve", lambda e, nh=nh: e.tensor_copy(out=gfin[:, sl(nh, 512)], in_=bk[1 + nh][:, 0:512]), reads=[("bk", 1 + nh)], writes=["gfin"])
    bank_ctr[0] = 3

    wg = {}

    def defgroup(name, nk, width, srcs):
        scr = nc.dram_tensor(f"scr_{name}", [128, nk, width], BF16, kind="Internal").ap()
        wg[name] = dict(nk=nk, width=width, srcs=srcs, scr=scr, conv=False)

    w_in_v = w_in.rearrange("(kc p) n -> p kc n", p=128)
    in_cols = [("in0", 0, 512), ("in1", 512, 512), ("in2", 1024, 512), ("in3", 1536, 16),
               ("in4", 1552, 512), ("in5", 2064, 512), ("in6", 2576, 512)]
    for name, c0, w in in_cols:
        defgroup(name, 8, w, [(0, w, w_in_v[:, :, c0:c0 + w])])
    w_out_v = w_out.rearrange("(kc p) n -> p kc n", p=128)
    w_pg_v = w_ple_gate.rearrange("(kc p) n -> p kc n", p=128)
    w_pp_v = w_ple_proj.rearrange("(kc p) n -> p kc n", p=128)
    for nh in range(2):
        defgroup(f"out{nh}", 8, 512, [(0, 512, w_out_v[:, :, sl(nh, 512)])])
        defgroup(f"pg{nh}", 8, 512, [(0, 512, w_pg_v[:, :, sl(nh, 512)])])
        defgroup(f"pp{nh}", 2, 512, [(0, 512, w_pp_v[:, :, sl(nh, 512)])])
    w_up_v = w_up.rearrange("(kc p) n -> p kc n", p=128)
    for g in range(11):
        defgroup(f"up{g}", 8, 512, [(0, 256, w_up_v[:, :, 256 * g:256 * g + 256]),
                                    (256, 256, w_up_v[:, :, DFF + 256 * g:DFF + 256 * g + 256])])
    w_dn_v = w_down.rearrange("(j p) n -> p j n", p=128)
    DN_GROUPS = [(0, 8), (8, 16), (16, 22)]
    for nh in range(2):
        for jg, (j0, j1) in enumerate(DN_GROUPS):
            defgroup(f"dn{nh}_{jg}", j1 - j0, 512, [(0, 512, w_dn_v[:, j0:j1, sl(nh, 512)])])

    ws_ctr = [0]
    st_ctr = [0]

    def wst_ids(st):
        return [("wst", st)] + [("act", j) for j in range(4 * st, 4 * st + 4)]

    def stage_and_cast(g, dst_tile, dst_ids):
        nk, width = g["nk"], g["width"]
        for k0 in range(0, nk, 4):
            k1 = min(nk, k0 + 4)
            st = st_ctr[0] % 2
            st_ctr[0] += 1
            for (dc, w, src) in g["srcs"]:
                P.add("sp", lambda e, st=st, k0=k0, k1=k1, dc=dc, w=w, src=src: e.dma_start(out=wstage[st][:, 0:k1 - k0, dc:dc + w], in_=src[:, k0:k1, :]),
                      writes=wst_ids(st), dsem=f"d_st{st}")
            P.add("pool", lambda e, st=st, k0=k0, k1=k1, width=width: e.tensor_copy(out=dst_tile[:, k0:k1, 0:width], in_=wstage[st][:, 0:k1 - k0, 0:width]),
                  reads=wst_ids(st), writes=dst_ids)

    def to_scratch(name, src_tile, src_ids):
        g = wg[name]
        nk, width = g["nk"], g["width"]
        P.add("pool", lambda e, nk=nk, width=width, scr=g["scr"]: e.dma_start(out=scr, in_=src_tile[:, 0:nk, 0:width]),
              reads=src_ids, writes=[("scr", name)], dsem=f"d_scr_{name}")
        g["conv"] = True

    held = set()

    def rel_w(*slots):
        for sl_ in slots:
            held.discard(sl_)

    def get_w(name):
        g = wg[name]
        while True:
            slot = ws_ctr[0] % NWS
            ws_ctr[0] += 1
            if slot not in held:
                break
        held.add(slot)
        nk, width = g["nk"], g["width"]
        while not g["conv"]:
            bg_step()
        assert g["conv"], name
        if True:
            P.add("sp", lambda e, slot=slot, nk=nk, width=width, scr=g["scr"]: e.dma_start(out=wslot[slot][:, 0:nk, 0:width], in_=scr),
                  reads=[("scr", name, k0) for k0 in range(0, nk, 2)], writes=[("ws", slot)], dsem=f"d_ws{slot}")
        return slot

    BG_ORDER = (["in1", "in3", "in0", "in2", "in5", "in6", "in4", "out0", "out1"] + [f"up{g}" for g in range(11)]
                + [f"dn{nh}_{jg}" for nh in range(2) for jg in range(3)] + ["pg0", "pp0", "pg1", "pp1"])
    NST = 5
    LOOK = 4

    def conv_gen():
        items = []
        for name in BG_ORDER:
            g = wg[name]
            for k0 in range(0, g["nk"], 2):
                items.append((name, k0, min(g["nk"], k0 + 2)))

        def load(i):
            name, k0, k1 = items[i]
            st = i % NST
            for (dc, w, src) in wg[name]["srcs"]:
                P.add("sp", lambda e, st=st, k0=k0, k1=k1, dc=dc, w=w, src=src: e.dma_start(out=wstage[st][:, 0:k1 - k0, dc:dc + w], in_=src[:, k0:k1, :]),
                      writes=wst_ids(st), dsem=f"d_st{st}")

        for i in range(min(LOOK, len(items))):
            load(i)
        for i, (name, k0, k1) in enumerate(items):
            st = i % NST
            q = i % 4
            width = wg[name]["width"]
            mids = [("mixT", 2 * q), ("mixT", 2 * q + 1)]
            P.add("pool", lambda e, st=st, k0=k0, k1=k1, q=q, width=width: e.tensor_copy(out=mixT[:, 2 * q:2 * q + k1 - k0, 0:width], in_=wstage[st][:, 0:k1 - k0, 0:width]),
                  reads=wst_ids(st), writes=mids)
            P.add("sp", lambda e, k0=k0, k1=k1, q=q, width=width, scr=wg[name]["scr"]: e.dma_start(out=scr[:, k0:k1, :], in_=mixT[:, 2 * q:2 * q + k1 - k0, 0:width]),
                  reads=mids, writes=[("scr", name, k0)], dsem=f"d_sw{q}")
            if i + LOOK < len(items):
                load(i + LOOK)
            if k1 == wg[name]["nk"]:
                wg[name]["conv"] = True
                yield "c"

    def cv(col):
        return cvec[:, col:col + 1]

    def rms_stats(hb, NS):
        h, ss, rs = hT[hb], ssT[hb], rsT[hb]
        for s in range(NS):
            P.add("act", lambda e, s=s: e.activation(out=junk[:], in_=h[:, s, :], func=AF.Square, accum_out=ss[:, s:s + 1]),
                  reads=[("h", hb, s)], writes=["junk", ("ss", hb, s)])
        P.add("dve", lambda e: e.tensor_scalar(out=rs[:, 0:NS], in0=ss[:, 0:NS], scalar1=1.0 / D, scalar2=EPS, op0=ALU.mult, op1=ALU.add),
              reads=[("ss", hb, s) for s in range(NS)], writes=[("rs", hb)])
        P.add("act", lambda e: e.activation(out=rs[:, 0:NS], in_=rs[:, 0:NS], func=AF.Sqrt), reads=[("rs", hb)], writes=[("rs", hb)])
        P.add("dve", lambda e: e.reciprocal(out=rs[:, 0:NS], in_=rs[:, 0:NS]), reads=[("rs", hb)], writes=[("rs", hb)])

    def norm_T(hb, NS, gcol):
        h, rs, yT, ybf = hT[hb], rsT[hb], yTT[hb], ybfT[hb]
        rms_stats(hb, NS)

        def scale(s):
            yb = s % 2
            P.add("act", lambda e, s=s, yb=yb: e.activation(out=ybf[yb][:], in_=h[:, s, :], func=AF.Copy, scale=rs[:, s:s + 1]),
                  reads=[("h", hb, s), ("rs", hb)], writes=[("ybf", hb, yb)])

        scale(0)
        yield "n"
        for s in range(NS):
            yb = s % 2
            b = nb()
            for kc in range(KC):
                P.add("pe", lambda e, b=b, kc=kc, yb=yb: e.transpose(out=bkb[b][:, sl(kc)], in_=ybf[yb][:, sl(kc)], identity=ident[:]),
                      reads=[("ybf", hb, yb), "ident"], writes=[("bk", b)])
            if s + 1 < NS:
                scale(s + 1)
            for kc in range(KC):
                if s % 2 == 0:
                    P.add("dve", lambda e, b=b, kc=kc, s=s: e.tensor_scalar(out=yT[:, kc, sl(s)], in0=bkb[b][:, sl(kc)], scalar1=cv(gcol + kc), scalar2=None, op0=ALU.mult),
                          reads=[("bk", b), "cvec"], writes=[("yT", hb, s)])
                else:
                    P.add("act", lambda e, b=b, kc=kc, s=s: e.activation(out=yT[:, kc, sl(s)], in_=bkb[b][:, sl(kc)], func=AF.Copy, scale=cv(gcol + kc)),
                          reads=[("bk", b), "cvec"], writes=[("yT", hb, s)])
            yield "n"

    def yT_ids(hb, NS):
        return [("yT", hb, s) for s in range(NS)]

    def proj_fm(hb, ws, c0, Tt, NS):
        yT = yTT[hb]
        b = nb()
        for kc in range(KC):
            P.add("pe", lambda e, b=b, kc=kc: e.matmul(bk[b][:, 0:Tt], lhsT=wslot[ws][:, kc, c0:c0 + 128], rhs=yT[:, kc, 0:Tt], start=(kc == 0), stop=(kc == KC - 1)),
                  reads=[("ws", ws)] + yT_ids(hb, NS), writes=[("bk", b)])
        return b

    def resid_add(hb, b, s, nh):
        h = hT[hb]
        P.add("dve", lambda e: e.tensor_tensor(out=h[:, s, sl(nh, 512)], in0=h[:, s, sl(nh, 512)], in1=bk[b][:, 0:512], op=ALU.add),
              reads=[("h", hb, s), ("bk", b)], writes=[("h", hb, s)])

    def tile(mode, src, r0, NS, hb, out_r0=None):
        Tt = NS * 128
        full = mode == "full"
        light = mode == "light"
        h, yT, rs = hT[hb], yTT[hb], rsT[hb]
        P.add("sp", lambda e: e.dma_start(out=h[:, 0:NS, :], in_=src[r0:r0 + Tt, :].rearrange("(s p) d -> p s d", p=128)),
              writes=[("h", hb, s) for s in range(NS)], dsem=f"d_x{hb}")
        yield from norm_T(hb, NS, C_ATTN)
        yield "A"
        ws = get_w("in1")
        for s in range(NS):
            b = nb()
            for kc in range(KC):
                P.add("pe", lambda e, b=b, kc=kc, s=s, ws=ws: e.matmul(bk[b][:, 0:512], lhsT=yT[:, kc, sl(s)], rhs=wslot[ws][:, kc, 0:512], start=(kc == 0), stop=(kc == KC - 1)),
                      reads=[("ws", ws), ("yT", hb, s)], writes=[("bk", b)])
            P.add("act", lambda e, b=b, s=s: e.activation(out=vtok[:, s, :], in_=bk[b][:, 0:512], func=AF.Copy),
                  reads=[("bk", b)], writes=[("vtok", s)])
            yield "c"
        rel_w(ws)
        if not light:
            ws = get_w("in2")
            for c in range(4):
                b = proj_fm(hb, ws, c * 128, Tt, NS)
                P.add("act", lambda e, b=b, c=c: e.activation(out=sg[:, c, 0:Tt], in_=bk[b][:, 0:Tt], func=AF.Silu),
                      reads=[("bk", b)], writes=[("sg", c)])
                yield "c"
        if not light:
            rel_w(ws)
        ws = get_w("in3")
        b = nb()
        for kc in range(KC):
            P.add("pe", lambda e, b=b, kc=kc, ws=ws: e.matmul(bk[b][0:16, 0:Tt], lhsT=wslot[ws][:, kc, 0:16], rhs=yT[:, kc, 0:Tt], start=(kc == 0), stop=(kc == KC - 1)),
                  reads=[("ws", ws)] + yT_ids(hb, NS), writes=[("bk", b)])
        P.add("dve", lambda e, b=b: e.tensor_copy(out=aaug[0:16, 0:Tt], in_=bk[b][0:16, 0:Tt]), reads=[("bk", b)], writes=["aaug"])
        rel_w(ws)
        yield "c"
        for s in range(NS):
            b = nb()
            P.add("pe", lambda e, b=b, s=s: e.matmul(bk[b][:, 0:256], lhsT=aaug[0:17, sl(s)], rhs=walpha[0:17, :], start=True, stop=True),
                  reads=["aaug", "walpha"], writes=[("bk", b)])
            xi = s % 2
            P.add("act", lambda e, b=b, xi=xi: e.activation(out=ex[xi][:], in_=bk[b][:, 0:256], func=AF.Exp, scale=-1.0),
                  reads=[("bk", b)], writes=[("ex", xi)])
            P.add("act", lambda e, s=s, xi=xi: e.activation(out=Lt[:, s, :], in_=ex[xi][:], func=AF.Ln, bias=one1[:, 0:1]),
                  reads=[("ex", xi), "one1"], writes=[("Lt", s)])
            if s % 2 == 1:
                yield "c"
        yield "c"
        for c in range(2):
            b = nb()
            for s in range(NS):
                P.add("pe", lambda e, b=b, s=s, c=c: e.matmul(bk[b][:, sl(s)], lhsT=Lt[:, s, sl(c)], rhs=trineg[:], start=True, stop=True),
                      reads=[("Lt", s), "trineg"], writes=[("bk", b)])
            P.add("act", lambda e, b=b, c=c: e.activation(out=eb[:, c, 0:Tt], in_=bk[b][:, 0:Tt], func=AF.Exp),
                  reads=[("bk", b)], writes=[("eb", c)])
            P.add("act", lambda e, b=b, c=c: e.activation(out=enb[:, c, 0:Tt], in_=bk[b][:, 0:Tt], func=AF.Exp, scale=-1.0),
                  reads=[("bk", b)], writes=[("enb", c)])
            yield "c"
        ws = get_w("in0")
        for c in range(2):
            if not light:
                b = proj_fm(hb, ws, c * 128, Tt, NS)
                for hh in range(2):
                    P.add("dve", lambda e, b=b, c=c, hh=hh: e.scalar_tensor_tensor(out=qdz[hh][:, c, 0:Tt], in0=bk[b][:, 0:Tt], scalar=hm[:, hh:hh + 1], in1=eb[:, c, 0:Tt], op0=ALU.mult, op1=ALU.mult),
                          reads=[("bk", b), ("eb", c), "hm"], writes=[("qd", c, hh)])
                yield "c"
            b = proj_fm(hb, ws, 256 + c * 128, Tt, NS)
            P.add("dve", lambda e, b=b, c=c: e.tensor_tensor(out=kd[:, c, 0:Tt], in0=bk[b][:, 0:Tt], in1=enb[:, c, 0:Tt], op=ALU.mult),
                  reads=[("bk", b), ("enb", c)], writes=[("kd", c)])
            yield "c"
        rel_w(ws)
        for s in range(NS):
            b = nb()
            for c in range(2):
                P.add("pe", lambda e, b=b, c=c, s=s: e.transpose(out=bkb[b][:, sl(c)], in_=kd[:, c, sl(s)], identity=ident[:]),
                      reads=[("kd", c), "ident"], writes=[("bk", b)])
            P.add("dve", lambda e, b=b, s=s: e.tensor_copy(out=kdtok[s][:], in_=bkb[b][:, 0:256]), reads=[("bk", b)], writes=[("kdtok", s)])
            if not light:
                b2 = nb()
                for hh4 in range(4):
                    c, hh = divmod(hh4, 2)
                    P.add("pe", lambda e, b2=b2, hh4=hh4, c=c, hh=hh, s=s: e.matmul(bk[b2][:, sl(hh4)], lhsT=kd[:, c, sl(s)], rhs=qdz[hh][:, c, sl(s)], start=True, stop=True),
                          reads=[("kd", c), ("qd", c, hh)], writes=[("bk", b2)])
                P.add("dve", lambda e, b2=b2, s=s: e.tensor_tensor(out=scm[s][:], in0=bk[b2][:, 0:512], in1=mtri4[:], op=ALU.mult),
                      reads=[("bk", b2), "mtri4"], writes=[("scm", s)])
            yield "c"
        for s in range(NS):
            if not light:
                b3 = nb()
                for hh4 in range(4):
                    c, hh = divmod(hh4, 2)
                    P.add("pe", lambda e, b3=b3, hh4=hh4, s=s: e.matmul(bk[b3][:, sl(hh4)], lhsT=vtok[:, s, sl(hh4)], rhs=scm[s][:, sl(hh4)], start=True, stop=False),
                          reads=[("vtok", s), ("scm", s)], writes=[("bk", b3)])
                    P.add("pe", lambda e, b3=b3, hh4=hh4, c=c, hh=hh, s=s: e.matmul(bk[b3][:, sl(hh4)], lhsT=Sbf[:, c, sl(hh)], rhs=qdz[hh][:, c, sl(s)], start=False, stop=True),
                          reads=[("Sbf", c), ("qd", c, hh)], writes=[("bk", b3)])
                P.add("act", lambda e, b3=b3, s=s: e.activation(out=oT[:, :, sl(s)], in_=bk[b3][:, 0:512].rearrange("p (a i) -> p a i", a=4), func=AF.Copy),
                      reads=[("bk", b3)], writes=[("oT", a) for a in range(4)])
                yield "c"
            for c in range(2):
                b4 = nb()
                P.add("pe", lambda e, b4=b4, c=c, s=s: e.matmul(bk[b4][:, 0:256], lhsT=kdtok[s][:, sl(c)], rhs=vtok[:, s, sl(c, 256)], start=True, stop=True),
                      reads=[("kdtok", s), ("vtok", s)], writes=[("bk", b4)])
                P.add("dve", lambda e, b4=b4, c=c: e.scalar_tensor_tensor(out=R[:, c, :], in0=R[:, c, :], scalar=dsv[:, c:c + 1], in1=bk[b4][:, 0:256], op0=ALU.mult, op1=ALU.add),
                      reads=[("R", c), ("dsv", c), ("bk", b4)], writes=[("R", c)])
                col = s * 128 + 127
                P.add("dve", lambda e, c=c, col=col: e.tensor_copy(out=dsv[:, c:c + 1], in_=eb[:, c, col:col + 1]),
                      reads=[("eb", c)], writes=[("dsv", c)])
                P.add("act", lambda e, c=c, col=col: e.activation(out=Sbf[:, c, :], in_=R[:, c, :], func=AF.Copy, scale=eb[:, c, col:col + 1]),
                      reads=[("R", c), ("eb", c)], writes=[("Sbf", c)])
            yield "c"
        if light:
            yield "M"
            return
        yield "H"
        for a in range(4):
            P.add("act", lambda e, a=a: e.activation(out=osq[:, a, 0:Tt], in_=oT[:, a, 0:Tt], func=AF.Square),
                  reads=[("oT", a)], writes=[("osq", a)])
        yield "c"
        hn = []
        for a in range(4):
            b = nb()
            P.add("pe", lambda e, b=b, a=a: e.matmul(bk[b][:, 0:Tt], lhsT=onesdv[:], rhs=osq[:, a, 0:Tt], start=True, stop=True),
                  reads=["onesdv", ("osq", a)], writes=[("bk", b)])
            t1 = nt()
            P.add("act", lambda e, b=b, t1=t1: e.activation(out=tmp[t1][:, 0:Tt], in_=bk[b][:, 0:Tt], func=AF.Sqrt, bias=epsT[:, 0:1]),
                  reads=[("bk", b), "epsT"], writes=[("tmp", t1)])
            P.add("dve", lambda e, t1=t1: e.reciprocal(out=tmp[t1][:, 0:Tt], in_=tmp[t1][:, 0:Tt]), reads=[("tmp", t1)], writes=[("tmp", t1)])
            P.add("dve", lambda e, t1=t1, a=a: e.tensor_tensor(out=tmp[t1][:, 0:Tt], in0=oT[:, a, 0:Tt], in1=tmp[t1][:, 0:Tt], op=ALU.mult),
                  reads=[("tmp", t1), ("oT", a)], writes=[("tmp", t1)])
            P.add("dve", lambda e, t1=t1, a=a: e.scalar_tensor_tensor(out=mixT[:, a, 0:Tt], in0=tmp[t1][:, 0:Tt], scalar=cv(C_HG + a), in1=sg[:, a, 0:Tt], op0=ALU.mult, op1=ALU.mult),
                  reads=[("tmp", t1), "cvec", ("sg", a)], writes=[("mixT", a)])
            yield "c"
        if not light:
            ws = get_w("in5")
            for c in range(4):
                b = proj_fm(hb, ws, c * 128, Tt, NS)
                P.add("act", lambda e, b=b, c=c: e.activation(out=oT[:, c, 0:Tt], in_=bk[b][:, 0:Tt], func=AF.Copy),
                      reads=[("bk", b)], writes=[("oT", c)])
                yield "c"
            rel_w(ws)
            ws = get_w("in6")
            for c in range(4):
                b = proj_fm(hb, ws, c * 128, Tt, NS)
                P.add("dve", lambda e, b=b, c=c: e.tensor_tensor(out=ubuf[:, c, 2:2 + Tt], in0=bk[b][:, 0:Tt], in1=oT[:, c, 0:Tt], op=ALU.mult),
                      reads=[("bk", b), ("oT", c)], writes=[("ub", c)])
                yield "c"
            rel_w(ws)
            ws = get_w("in4")
            for c in range(4):
                b = proj_fm(hb, ws, c * 128, Tt, NS)
                t1 = nt()
                P.add("act", lambda e, c=c, t1=t1: e.activation(out=tmp[t1][:, 0:Tt], in_=ubuf[:, c, 0:Tt], func=AF.Copy, scale=cv(C_MCW + 0 * 4 + c)),
                      reads=[("ub", c), "ubh", "cvec"], writes=[("tmp", t1)])
                P.add("dve", lambda e, c=c, t1=t1: e.scalar_tensor_tensor(out=tmp[t1][:, 0:Tt], in0=ubuf[:, c, 1:1 + Tt], scalar=cv(C_MCW + 1 * 4 + c), in1=tmp[t1][:, 0:Tt], op0=ALU.mult, op1=ALU.add),
                      reads=[("ub", c), "ubh", "cvec", ("tmp", t1)], writes=[("tmp", t1)])
                P.add("dve", lambda e, c=c, t1=t1: e.scalar_tensor_tensor(out=tmp[t1][:, 0:Tt], in0=ubuf[:, c, 2:2 + Tt], scalar=cv(C_MCW + 2 * 4 + c), in1=tmp[t1][:, 0:Tt], op0=ALU.mult, op1=ALU.add),
                      reads=[("ub", c), "cvec", ("tmp", t1)], writes=[("tmp", t1)])
                P.add("dve", lambda e, c=c, t1=t1, b=b: e.tensor_tensor(out=mixT[:, 4 + c, 0:Tt], in0=tmp[t1][:, 0:Tt], in1=bk[b][:, 0:Tt], op=ALU.mult),
                      reads=[("tmp", t1), ("bk", b)], writes=[("mixT", 4 + c)])
                yield "c"
            rel_w(ws)
            P.add("pool", lambda e: e.tensor_copy(out=ubuf[:, :, 0:2], in_=ubuf[:, :, Tt:Tt + 2]),
                  reads=[("ub", c) for c in range(4)], writes=["ubh"])
        yield "M"
        for nh in range(2):
            ws = get_w(f"out{nh}")
            for s in range(NS):
                b = nb()
                for kc in range(KC):
                    P.add("pe", lambda e, b=b, kc=kc, s=s, ws=ws: e.matmul(bk[b][:, 0:512], lhsT=mixT[:, kc, sl(s)], rhs=wslot[ws][:, kc, 0:512], start=(kc == 0), stop=(kc == KC - 1)),
                          reads=[("ws", ws), ("mixT", kc)], writes=[("bk", b)])
                resid_add(hb, b, s, nh)
                yield "c"
            rel_w(ws)
        yield "W"
        if full:
            P.add("sp", lambda e: e.dma_start(out=pbuf[:, 0:NS, :], in_=p_d[r0:r0 + Tt, :].rearrange("(s p) d -> p s d", p=128)),
                  writes=["pbuf"], dsem="d_p")
            P.add("pool", lambda e: e.tensor_copy(out=pbf[:, 0:NS, :], in_=pbuf[:, 0:NS, :]), reads=["pbuf"], writes=["pbf"])
        yield from norm_T(hb, NS, C_FFN)
        ws = None
        for g in range(11):
            if ws is not None:
                rel_w(ws)
            ws = get_w(f"up{g}")
            for jj in range(2):
                j = 2 * g + jj
                bg = proj_fm(hb, ws, jj * 128, Tt, NS)
                gi = j % 3
                P.add("pool", lambda e, gi=gi, j=j: e.tensor_copy(out=gbuf[gi][:, 0:2], in_=ghalo[:, j, :]),
                      reads=[("gh", j)], writes=[("gbh", gi)])
                P.add("act", lambda e, gi=gi, bg=bg: e.activation(out=gbuf[gi][:, 2:2 + Tt], in_=bk[bg][:, 0:Tt], func=AF.Copy),
                      reads=[("bk", bg)], writes=[("gb", gi)])
                P.add("pool", lambda e, gi=gi, j=j: e.tensor_copy(out=ghalo[:, j, :], in_=gbuf[gi][:, Tt:Tt + 2]),
                      reads=[("gb", gi)], writes=[("gh", j)])
                if not full:
                    yield "c"
                    continue
                bu = proj_fm(hb, ws, 256 + jj * 128, Tt, NS)
                t1 = nt()
                P.add("act", lambda e, gi=gi, j=j, t1=t1: e.activation(out=tmp[t1][:, 0:Tt], in_=gbuf[gi][:, 0:Tt], func=AF.Copy, scale=cv(C_FCW + 0 * NJ + j)),
                      reads=[("gb", gi), ("gbh", gi), "cvec"], writes=[("tmp", t1)])
                P.add("dve", lambda e, gi=gi, j=j, t1=t1: e.scalar_tensor_tensor(out=tmp[t1][:, 0:Tt], in0=gbuf[gi][:, 1:1 + Tt], scalar=cv(C_FCW + 1 * NJ + j), in1=tmp[t1][:, 0:Tt], op0=ALU.mult, op1=ALU.add),
                      reads=[("gb", gi), ("gbh", gi), "cvec", ("tmp", t1)], writes=[("tmp", t1)])
                P.add("dve", lambda e, gi=gi, j=j, t1=t1: e.scalar_tensor_tensor(out=tmp[t1][:, 0:Tt], in0=gbuf[gi][:, 2:2 + Tt], scalar=cv(C_FCW + 2 * NJ + j), in1=tmp[t1][:, 0:Tt], op0=ALU.mult, op1=ALU.add),
                      reads=[("gb", gi), "cvec", ("tmp", t1)], writes=[("tmp", t1)])
                P.add("act", lambda e, t1=t1: e.activation(out=tmp[t1][:, 0:Tt], in_=tmp[t1][:, 0:Tt], func=AF.Silu),
                      reads=[("tmp", t1)], writes=[("tmp", t1)])
                P.add("dve", lambda e, t1=t1, j=j, bu=bu: e.tensor_tensor(out=actT[:, j, 0:Tt], in0=tmp[t1][:, 0:Tt], in1=bk[bu][:, 0:Tt], op=ALU.mult),
                      reads=[("tmp", t1), ("bk", bu)], writes=[("act", j)])
                yield "c"
        rel_w(ws)
        if not full:
            return
        for nh in range(2):
            banks4 = reserve(NS)
            for jg, (j0, j1) in enumerate(DN_GROUPS):
                ws = get_w(f"dn{nh}_{jg}")
                for s in range(NS):
                    for j in range(j0, j1):
                        P.add("pe", lambda e, b=banks4[s], j=j, j0=j0, s=s, ws=ws: e.matmul(bk[b][:, 0:512], lhsT=actT[:, j, sl(s)], rhs=wslot[ws][:, j - j0, 0:512], start=(j == 0), stop=(j == NJ - 1)),
                              reads=[("ws", ws), ("act", j)], writes=[("bk", banks4[s])])
                    yield "c"
                rel_w(ws)
            for s in range(NS):
                resid_add(hb, banks4[s], s, nh)
            release(banks4)
            yield "c"
        yield from norm_T(hb, NS, C_PLE)
        for s in range(NS):
            b = nb()
            for kc in range(2):
                P.add("pe", lambda e, b=b, kc=kc, s=s: e.transpose(out=bkb[b][:, sl(kc)], in_=pbf[:, s, sl(kc)], identity=ident[:]),
                      reads=["pbf", "ident"], writes=[("bk", b)])
            P.add("dve", lambda e, b=b, s=s: e.tensor_copy(out=pT[:, :, sl(s)], in_=bkb[b][:, 0:256].rearrange("p (a i) -> p a i", a=2)),
                  reads=[("bk", b)], writes=[("pT", s)])
        yield "c"
        for nh in range(2):
            wsg = get_w(f"pg{nh}")
            wsp = get_w(f"pp{nh}")
            for s in range(NS):
                bg = nb()
                for kc in range(KC):
                    P.add("pe", lambda e, bg=bg, kc=kc, s=s, wsg=wsg: e.matmul(bk[bg][:, 0:512], lhsT=yT[:, kc, sl(s)], rhs=wslot[wsg][:, kc, 0:512], start=(kc == 0), stop=(kc == KC - 1)),
                          reads=[("ws", wsg), ("yT", hb, s)], writes=[("bk", bg)])
                bp = nb()
                for kc in range(2):
                    P.add("pe", lambda e, bp=bp, kc=kc, s=s, wsp=wsp: e.matmul(bk[bp][:, 0:512], lhsT=pT[:, kc, sl(s)], rhs=wslot[wsp][:, kc, 0:512], start=(kc == 0), stop=(kc == 1)),
                          reads=[("ws", wsp), ("pT", s)], writes=[("bk", bp)])
                t1 = nt()
                P.add("act", lambda e, t1=t1, bg=bg: e.activation(out=tmp[t1][:], in_=bk[bg][:, 0:512], func=AF.Sigmoid),
                      reads=[("bk", bg)], writes=[("tmp", t1)])
                P.add("dve", lambda e, t1=t1, bp=bp: e.tensor_tensor(out=tmp[t1][:], in0=tmp[t1][:], in1=bk[bp][:, 0:512], op=ALU.mult),
                      reads=[("tmp", t1), ("bk", bp)], writes=[("tmp", t1)])
                P.add("pool", lambda e, t1=t1, s=s, nh=nh: e.tensor_tensor(out=h[:, s, sl(nh, 512)], in0=h[:, s, sl(nh, 512)], in1=tmp[t1][:], op=ALU.add),
                      reads=[("tmp", t1), ("h", hb, s)], writes=[("h", hb, s)])
                yield "c"
            rel_w(wsg, wsp)
        rms_stats(hb, NS)
        yield "c"
        for s in range(NS):
            P.add("dve", lambda e, s=s: e.scalar_tensor_tensor(out=h[:, s, :], in0=h[:, s, :], scalar=rs[:, s:s + 1], in1=gfin[:], op0=ALU.mult, op1=ALU.mult),
                  reads=[("h", hb, s), ("rs", hb), "gfin"], writes=[("h", hb, s)])
            P.add("pool", lambda e, s=s: e.dma_start(out=out_d[out_r0 + s * 128:out_r0 + (s + 1) * 128, :], in_=h[:, s, :]),
                  reads=[("h", hb, s)], writes=[("o", out_r0, s)], dsem=f"d_o{hb}")
            yield "c"

    specs = []
    r = 0
    nl = NL
    while nl > 0:
        ns = min(4, nl)
        specs.append(("light", xp_d, r, ns, None))
        r += ns * 128
        nl -= ns
    if HALO:
        specs.append(("halo", xp_d, r, 1, None))
    out_ids = []
    for t in range(NF):
        specs.append(("full", x_d, t * 512, 4, t * 512))
        out_ids += [("o", t * 512, s) for s in range(4)]
    runs = []
    for i, (mode, src, r0, ns, o0) in enumerate(specs):
        runs.append(dict(g=tile(mode, src, r0, ns, i % 2, out_r0=o0), tag=None, fin=False, mode=mode))
    bg = dict(g=conv_gen(), fin=False)

    def bg_step():
        if not bg["fin"]:
            try:
                next(bg["g"])
            except StopIteration:
                bg["fin"] = True

    def one(t):
        try:
            t["tag"] = next(t["g"])
        except StopIteration:
            t["fin"] = True
            t["tag"] = "END"

    def reached(t, targets):
        return t is None or t["fin"] or t["tag"] in targets

    nstep = [0]

    def interleave(a, ta, b, tb, ra=2, rb=1):
        while not (reached(a, ta) and reached(b, tb)):
            for _ in range(ra):
                if reached(a, ta):
                    break
                one(a)
            for _ in range(rb):
                if reached(b, tb):
                    break
                one(b)
            nstep[0] += 1
            if nstep[0] % 2 == 0:
                bg_step()

    def drain_bg():
        while not bg["fin"]:
            bg_step()

    if runs[0]["mode"] != "light":
        drain_bg()
    while not wg["in1"]["conv"]:
        bg_step()
    for i, cur in enumerate(runs):
        nxt = runs[i + 1] if i + 1 < len(runs) else None
        if nxt is not None and nxt["mode"] != "light":
            drain_bg()
        if cur["mode"] == "light":
            interleave(cur, {"W"}, nxt, {"A"})
        else:
            interleave(cur, {"W"}, None, {"A"})
        credit = 0.0
        while not reached(cur, {"END"}):
            one(cur)
            credit += 3.0 if cur["tag"] == "n" else 0.6
            while credit >= 1.0 and not reached(nxt, {"M"}):
                one(nxt)
                credit -= 1.0
            if reached(nxt, {"M"}):
                credit = 0.0
            nstep[0] += 1
            if nstep[0] % 2 == 0:
                bg_step()
    while not bg["fin"]:
        bg_step()
    P.add("sp", None, reads=out_ids)

    stats = P.analyze()
    keys = P.sem_keys()
    sems = {k: es.enter_context(nc.semaphore(f"s{i}")) for i, k in enumerate(keys)}
    with nc.Block() as block:
        P.emit(block, sems)
    es.close()
    return nc, stats


_NAMES = ["attn_norm", "w_in", "w_alpha_up", "b_alpha", "gla_head_norm", "mix_conv_w", "w_out", "ffn_norm",
          "w_up", "ffn_conv_w", "w_down", "ple_norm", "w_ple_gate", "w_ple_proj"]


def make_in_maps(inputs, n_cores=8):
    x = np.asarray(inputs["x"], dtype=np.float32)
    p = np.asarray(inputs["p"], dtype=np.float32)
    common = {k: np.ascontiguousarray(np.asarray(inputs[k], dtype=np.float32)[0]) for k in _NAMES}
    common["final_norm"] = np.ascontiguousarray(np.asarray(inputs["final_norm"], dtype=np.float32))
    maps = []
    for c in range(n_cores):
        b, hf = divmod(c, 2)
        m = dict(common)
        m["x"] = np.ascontiguousarray(x[b, hf * SEQ_HALF:(hf + 1) * SEQ_HALF])
        m["p"] = np.ascontiguousarray(p[0, b, hf * SEQ_HALF:(hf + 1) * SEQ_HALF])
        if hf == 1:
            m["xp"] = np.ascontiguousarray(x[b, 0:SEQ_HALF])
        else:
            m["xp"] = np.zeros((SEQ_HALF, D), np.float32)
        maps.append(m)
    return maps


def kernel(**inputs):
    nc, _ = build_nc()
    maps = make_in_maps(inputs)
    res = run_bass_kernel_spmd(nc, maps, core_ids=list(range(8)))
    x = inputs["x"]
    out = np.empty((4, 2 * SEQ_HALF, D), np.float32)
    for c in range(8):
        b, hf = divmod(c, 2)
        out[b, hf * SEQ_HALF:(hf + 1) * SEQ_HALF] = np.asarray(res.results[c]["out"], dtype=np.float32)
    return out
```

```python
import numpy as np
from contextlib import ExitStack
import concourse.bass as bass
import concourse.mybir as mybir
from concourse.bass_utils import run_bass_kernel_spmd

F32 = mybir.dt.float32
BF16 = mybir.dt.bfloat16
AF = mybir.ActivationFunctionType
ALU = mybir.AluOpType

ENGS = ("pe", "act", "dve", "pool", "sp")
SAME_ENG_RAW_WINDOW = 4
STRICT_SAME_ENGINE = True

D = 1024
KC = 8
DIN = 3088
DFF = 2816
NJ = 22
PLE = 256
EPS = 1e-6
SEQ_HALF = 4096


class Op:
    __slots__ = ("eng", "fn", "reads", "writes", "dsem", "idx", "waits", "inc", "val", "pos")


class Prog:
    def __init__(self):
        self.ops = []

    def add(self, eng, fn, reads=(), writes=(), dsem=None):
        o = Op()
        o.eng = eng
        o.fn = fn
        o.reads = tuple(reads)
        o.writes = tuple(writes)
        o.dsem = dsem
        o.idx = len(self.ops)
        o.waits = {}
        o.inc = False
        o.val = None
        o.pos = None
        self.ops.append(o)
        return o

    def analyze(self):
        last_w = {}
        readers = {}
        pos = {e: 0 for e in ENGS}
        dcount = {}
        deps_of = []
        for o in self.ops:
            o.pos = pos[o.eng]
            pos[o.eng] += 1
            if o.dsem is not None:
                dcount[o.dsem] = dcount.get(o.dsem, 0) + 16
                o.val = dcount[o.dsem]
            deps = {}
            for b in o.reads:
                for w in last_w.get(b, ()):
                    deps[w.idx] = (w, "raw")
            for b in o.writes:
                for w in last_w.get(b, ()):
                    if o.dsem is not None and w.dsem == o.dsem and not readers.get(b):
                        continue
                    if w.idx not in deps:
                        deps[w.idx] = (w, "waw")
                for r in readers.get(b, ()):
                    if r.idx not in deps:
                        deps[r.idx] = (r, "war")
            keep = []
            for w, kind in deps.values():
                if w is o:
                    continue
                if w.dsem is None and o.dsem is None and w.eng == o.eng:
                    if w.eng == "pe":
                        continue
                    if not STRICT_SAME_ENGINE:
                        if kind != "raw":
                            continue
                        if o.pos - w.pos > SAME_ENG_RAW_WINDOW:
                            continue
                keep.append(w)
            best = {}
            rest = []
            for w in keep:
                if w.dsem is None:
                    if w.eng not in best or best[w.eng].pos < w.pos:
                        best[w.eng] = w
                else:
                    rest.append(w)
            keep = rest + list(best.values())
            deps_of.append(keep)
            for b in o.reads:
                readers.setdefault(b, []).append(o)
            for b in o.writes:
                lw = last_w.get(b)
                if o.dsem is not None and lw and lw[0].dsem == o.dsem and not readers.get(b):
                    lw.append(o)
                else:
                    last_w[b] = [o]
                readers[b] = []
        for keep in deps_of:
            for w in keep:
                if w.dsem is None:
                    w.inc = True
        cnt = {e: 0 for e in ENGS}
        for o in self.ops:
            if o.dsem is None and o.inc:
                cnt[o.eng] += 1
                o.val = cnt[o.eng]
        waited = {e: {} for e in ENGS}
        nw = 0
        for o, keep in zip(self.ops, deps_of):
            need = {}
            for w in keep:
                key = w.dsem if w.dsem is not None else ("eng", w.eng)
                need[key] = max(need.get(key, 0), w.val)
            wd = waited[o.eng]
            for key, v in need.items():
                if wd.get(key, 0) >= v:
                    continue
                wd[key] = v
                o.waits[key] = v
                nw += 1
        self.stats = dict(nops=len(self.ops), nwaits=nw, incs=dict(cnt), dma=dict(dcount))
        return self.stats

    def emit(self, block, sems):
        by_eng = {e: [o for o in self.ops if o.eng == e] for e in ENGS}

        def body(ename):
            def run(eng):
                for o in by_eng[ename]:
                    for key, v in o.waits.items():
                        eng.wait_ge(sems[key], v)
                    if o.fn is None:
                        continue
                    ins = o.fn(eng)
                    if o.dsem is not None:
                        ins.then_inc(sems[o.dsem], 16)
                    elif o.inc:
                        ins.then_inc(sems[("eng", ename)], 1)
            return run

        block.tensor(body("pe"))
        block.scalar(body("act"))
        block.vector(body("dve"))
        block.gpsimd(body("pool"))
        block.sync(body("sp"))

    def sem_keys(self):
        keys = [("eng", e) for e in ENGS if e != "sp"]
        seen = set()
        for o in self.ops:
            if o.dsem is not None and o.dsem not in seen:
                seen.add(o.dsem)
                keys.append(o.dsem)
        return keys


def sl(s, n=128):
    return slice(s * n, (s + 1) * n)


def build_nc(NL=31, NF=8, HALO=True):
    NPRE = NL * 128 + (128 if HALO else 0)
    NTOK = NF * 512
    nc = bass.Bass("TRN2", target_bir_lowering=False)

    def din(name, shape):
        return nc.dram_tensor(name, list(shape), F32, kind="ExternalInput").ap()

    xp_d = din("xp", [max(NPRE, 128), D])
    x_d = din("x", [NTOK, D])
    p_d = din("p", [NTOK, PLE])
    attn_norm = din("attn_norm", [D])
    w_in = din("w_in", [D, DIN])
    w_alpha_up = din("w_alpha_up", [16, 256])
    b_alpha = din("b_alpha", [256])
    gla_head_norm = din("gla_head_norm", [4, 128])
    mix_conv_w = din("mix_conv_w", [3, 512])
    w_out = din("w_out", [D, D])
    ffn_norm = din("ffn_norm", [D])
    w_up = din("w_up", [D, 2 * DFF])
    ffn_conv_w = din("ffn_conv_w", [3, DFF])
    w_down = din("w_down", [DFF, D])
    ple_norm = din("ple_norm", [D])
    w_ple_gate = din("w_ple_gate", [D, D])
    w_ple_proj = din("w_ple_proj", [PLE, D])
    final_norm = din("final_norm", [D])
    out_d = nc.dram_tensor("out", [NTOK, D], F32, kind="ExternalOutput").ap()

    P = Prog()
    es = ExitStack()

    def sb(name, shape, dt):
        return es.enter_context(nc.sbuf_tensor(name, list(shape), dt))

    hT = [sb(f"h{i}", [128, 4, D], F32) for i in range(2)]
    ybfT = [[sb(f"ybf{p}_{i}", [128, D], BF16) for i in range(2)] for p in range(2)]
    junk = sb("junk", [128, D], BF16)
    ssT = [sb(f"ss{i}", [128, 4], F32) for i in range(2)]
    rsT = [sb(f"rs{i}", [128, 4], F32) for i in range(2)]
    yTT = [sb(f"yT{i}", [128, KC, 512], BF16) for i in range(2)]
    mixT = sb("mixT", [128, KC, 512], BF16)
    big = sb("big", [128, NJ * 256], F32)
    actT = big[:].bitcast(BF16).rearrange("p (j t) -> p j t", j=NJ)
    vtok = sb("vtok", [128, 4, 512], BF16)
    sg = sb("sg", [128, 4, 512], BF16)
    qdz = [sb(f"qdz{i}", [128, 2, 512], BF16) for i in range(2)]
    hm = sb("hm", [128, 2], F32)
    kd = sb("kd", [128, 2, 512], BF16)
    eb = sb("eb", [128, 2, 512], F32)
    enb = sb("enb", [128, 2, 512], BF16)
    Lt = sb("Lt", [128, 4, 256], BF16)
    ex = [sb(f"ex{i}", [128, 256], F32) for i in range(2)]
    aaug = sb("aaug", [32, 512], BF16)
    kdtok = [sb(f"kdtok{i}", [128, 256], BF16) for i in range(4)]
    scm = [sb(f"scm{i}", [128, 512], BF16) for i in range(4)]
    oT = sb("oT", [128, 4, 512], F32)
    osq = sb("osq", [128, 4, 512], BF16)
    R = sb("R", [128, 2, 256], F32)
    Sbf = sb("Sbf", [128, 2, 256], BF16)
    dsv = sb("dsv", [128, 2], F32)
    ubuf = sb("ubuf", [128, 4, 514], F32)
    NTMP = 4
    tmp = [sb(f"tmp{i}", [128, 512], F32) for i in range(NTMP)]
    gbuf = [sb(f"gbuf{i}", [128, 514], F32) for i in range(3)]
    ghalo = sb("ghalo", [128, NJ, 2], F32)
    pbuf = sb("pbuf", [128, 4, PLE], F32)
    pbf = sb("pbf", [128, 4, PLE], BF16)
    pT = sb("pT", [128, 2, 512], BF16)
    NWS = 4
    wslot = [sb(f"wslot{i}", [128, KC, 512], BF16) for i in range(NWS)]
    wstage = [big[:, i * 1024:(i + 1) * 1024].rearrange("p (a b) -> p a b", a=2) for i in range(5)]
    onesf = sb("onesf", [128, 512], F32)
    ident = sb("ident", [128, 128], BF16)
    identf = sb("identf", [128, 128], F32)
    mtri4 = sb("mtri4", [128, 512], BF16)
    negf = sb("negf", [128, 128], F32)
    trineg = sb("trineg", [128, 128], BF16)
    onesdv = sb("onesdv", [128, 128], BF16)
    epsT = sb("epsT", [128, 1], F32)
    one1 = sb("one1", [128, 1], F32)
    craw = sb("craw", [128, 128], F32)
    cvec = sb("cvec", [128, 128], F32)
    gfin = sb("gfin", [128, D], F32)
    walpha = sb("walpha", [32, 256], BF16)

    bk = [es.enter_context(nc.psum_tensor(f"bk{i}", [128, 512], F32)) for i in range(8)]
    bkb = [b[:].bitcast(BF16) for b in bk]
    bank_ctr = [0]

    reserved = set()

    def nb():
        while True:
            i = bank_ctr[0] % 8
            bank_ctr[0] += 1
            if i not in reserved:
                return i

    def reserve(n):
        got = [nb() for _ in range(n)]
        reserved.update(got)
        return got

    def release(bs):
        reserved.difference_update(bs)

    tmp_ctr = [0]

    def nt():
        i = tmp_ctr[0] % NTMP
        tmp_ctr[0] += 1
        return i

    C_ATTN, C_FFN, C_PLE, C_HG, C_MCW, C_FCW = 0, 8, 16, 24, 28, 40

    P.add("pool", lambda e: e.memset(onesf[:], 1.0), writes=["onesf"])
    P.add("pool", lambda e: e.affine_select(out=ident[:], in_=onesf[:, 0:128], pattern=[[-1, 128]], compare_op=ALU.is_equal, fill=0.0, base=0, channel_multiplier=1), reads=["onesf"], writes=["ident"])
    P.add("pool", lambda e: e.affine_select(out=identf[:], in_=onesf[:, 0:128], pattern=[[-1, 128]], compare_op=ALU.is_equal, fill=0.0, base=0, channel_multiplier=1), reads=["onesf"], writes=["identf"])
    P.add("pool", lambda e: e.affine_select(out=mtri4[:].rearrange("p (h i) -> p h i", h=4), in_=onesf[:].rearrange("p (h i) -> p h i", h=4), pattern=[[0, 4], [1, 128]], compare_op=ALU.is_ge, fill=0.0, base=0, channel_multiplier=-1), reads=["onesf"], writes=["mtri4"])
    P.add("pool", lambda e: e.memset(negf[:], -0.0625), writes=["negf"])
    P.add("pool", lambda e: e.affine_select(out=trineg[:], in_=negf[:], pattern=[[1, 128]], compare_op=ALU.is_ge, fill=0.0, base=0, channel_multiplier=-1), reads=["negf"], writes=["trineg"])
    P.add("pool", lambda e: e.memset(onesdv[:], 1.0 / 128.0), writes=["onesdv"])
    P.add("pool", lambda e: e.memset(epsT[:], EPS), writes=["epsT"])
    P.add("pool", lambda e: e.memset(one1[:], 1.0), writes=["one1"])
    P.add("pool", lambda e: e.memset(aaug[:], 1.0), writes=["aaug"])
    P.add("pool", lambda e: e.memset(hm[:], 0.0), writes=["hm"])
    P.add("pool", lambda e: e.memset(hm[0:64, 0:1], 0.125), writes=["hm"])
    P.add("pool", lambda e: e.memset(hm[64:128, 1:2], 0.125), writes=["hm"])
    P.add("pool", lambda e: e.memset(R[:], 0.0), writes=[("R", 0), ("R", 1)])
    P.add("pool", lambda e: e.memset(Sbf[:], 0.0), writes=[("Sbf", 0), ("Sbf", 1)])
    P.add("pool", lambda e: e.memset(dsv[:], 1.0), writes=[("dsv", 0), ("dsv", 1)])
    P.add("pool", lambda e: e.memset(ubuf[:], 0.0), writes=[("ub", c) for c in range(4)] + ["ubh"])
    P.add("pool", lambda e: e.memset(ghalo[:], 0.0), writes=[("gh", j) for j in range(NJ)])
    P.add("pool", lambda e: e.memset(craw[:], 0.0), writes=["craw"])
    crows = [
        (C_ATTN, 8, attn_norm.rearrange("(c p) -> c p", p=128)),
        (C_FFN, 8, ffn_norm.rearrange("(c p) -> c p", p=128)),
        (C_PLE, 8, ple_norm.rearrange("(c p) -> c p", p=128)),
        (C_HG, 4, gla_head_norm),
        (C_MCW, 12, mix_conv_w.rearrange("k (c p) -> (k c) p", p=128)),
        (C_FCW, 66, ffn_conv_w.rearrange("k (j p) -> (k j) p", p=128)),
    ]
    for r0, n, src in crows:
        P.add("sp", lambda e, r0=r0, n=n, src=src: e.dma_start(out=craw[r0:r0 + n, :], in_=src), reads=[], writes=["craw"], dsem="d_c1")
    P.add("pe", lambda e: e.transpose(out=bk[0][:, 0:128], in_=craw[:, :], identity=identf[:]), reads=["craw", "identf"], writes=[("bk", 0)])
    P.add("dve", lambda e: e.tensor_copy(out=cvec[:], in_=bk[0][:, 0:128]), reads=[("bk", 0)], writes=["cvec"])
    P.add("pool", lambda e: e.memset(tmp[1][0:32, 0:256], 0.0), writes=["wa_st", ("tmp", 1)])
    P.add("sp", lambda e: e.dma_start(out=tmp[1][0:16, 0:256], in_=w_alpha_up), writes=["wa_st", ("tmp", 1)], dsem="d_c2")
    P.add("sp", lambda e: e.dma_start(out=tmp[1][16:17, 0:256], in_=b_alpha.rearrange("(o n) -> o n", o=1)), writes=["wa_st", ("tmp", 1)], dsem="d_c2")
    P.add("pool", lambda e: e.tensor_copy(out=walpha[:], in_=tmp[1][0:32, 0:256]), reads=["wa_st", ("tmp", 1)], writes=["walpha"])
    for nh in range(2):
        P.add("sp", lambda e, nh=nh: e.dma_start(out=tmp[2 * nh][0:1, :], in_=final_norm[sl(nh, 512)].rearrange("(o n) -> o n", o=1)), writes=[("gfrow", nh), ("tmp", 2 * nh)], dsem=f"d_c3{nh}")
    for nh in range(2):
        P.add("pe", lambda e, nh=nh: e.matmul(bk[1 + nh][:, 0:512], lhsT=onesf[0:1, 0:128], rhs=tmp[2 * nh][0:1, :], start=True, stop=True), reads=["onesf", ("gfrow", nh), ("tmp", 2 * nh)], writes=[("bk", 1 + nh)])
        P.add("dve", lambda e, nh=nh: e.tensor_copy(out=gfin[:, sl(nh, 512)], in_=bk[1 + nh][:, 0:512]), reads=[("bk", 1 + nh)], writes=["gfin"])
    bank_ctr[0] = 3

    wg = {}

    def defgroup(name, nk, width, srcs):
        scr = nc.dram_tensor(f"scr_{name}", [128, nk, width], BF16, kind="Internal").ap()
        wg[name] = dict(nk=nk, width=width, srcs=srcs, scr=scr, conv=False)

    w_in_v = w_in.rearrange("(kc p) n -> p kc n", p=128)
    in_cols = [("in0", 0, 512), ("in1", 512, 512), ("in2", 1024, 512), ("in3", 1536, 16),
               ("in4", 1552, 512), ("in5", 2064, 512), ("in6", 2576, 512)]
    for name, c0, w in in_cols:
        defgroup(name, 8, w, [(0, w, w_in_v[:, :, c0:c0 + w])])
    w_out_v = w_out.rearrange("(kc p) n -> p kc n", p=128)
    w_pg_v = w_ple_gate.rearrange("(kc p) n -> p kc n", p=128)
    w_pp_v = w_ple_proj.rearrange("(kc p) n -> p kc n", p=128)
    for nh in range(2):
        defgroup(f"out{nh}", 8, 512, [(0, 512, w_out_v[:, :, sl(nh, 512)])])
        defgroup(f"pg{nh}", 8, 512, [(0, 512, w_pg_v[:, :, sl(nh, 512)])])
        defgroup(f"pp{nh}", 2, 512, [(0, 512, w_pp_v[:, :, sl(nh, 512)])])
    w_up_v = w_up.rearrange("(kc p) n -> p kc n", p=128)
    for g in range(11):
        defgroup(f"up{g}", 8, 512, [(0, 256, w_up_v[:, :, 256 * g:256 * g + 256]),
                                    (256, 256, w_up_v[:, :, DFF + 256 * g:DFF + 256 * g + 256])])
    w_dn_v = w_down.rearrange("(j p) n -> p j n", p=128)
    DN_GROUPS = [(0, 8), (8, 16), (16, 22)]
    for nh in range(2):
        for jg, (j0, j1) in enumerate(DN_GROUPS):
            defgroup(f"dn{nh}_{jg}", j1 - j0, 512, [(0, 512, w_dn_v[:, j0:j1, sl(nh, 512)])])

    ws_ctr = [0]
    st_ctr = [0]

    def wst_ids(st):
        return [("wst", st)] + [("act", j) for j in range(4 * st, 4 * st + 4)]

    def stage_and_cast(g, dst_tile, dst_ids):
        nk, width = g["nk"], g["width"]
        for k0 in range(0, nk, 4):
            k1 = min(nk, k0 + 4)
            st = st_ctr[0] % 2
            st_ctr[0] += 1
            for (dc, w, src) in g["srcs"]:
                P.add("sp", lambda e, st=st, k0=k0, k1=k1, dc=dc, w=w, src=src: e.dma_start(out=wstage[st][:, 0:k1 - k0, dc:dc + w], in_=src[:, k0:k1, :]),
                      writes=wst_ids(st), dsem=f"d_st{st}")
            P.add("pool", lambda e, st=st, k0=k0, k1=k1, width=width: e.tensor_copy(out=dst_tile[:, k0:k1, 0:width], in_=wstage[st][:, 0:k1 - k0, 0:width]),
                  reads=wst_ids(st), writes=dst_ids)

    def to_scratch(name, src_tile, src_ids):
        g = wg[name]
        nk, width = g["nk"], g["width"]
        P.add("pool", lambda e, nk=nk, width=width, scr=g["scr"]: e.dma_start(out=scr, in_=src_tile[:, 0:nk, 0:width]),
              reads=src_ids, writes=[("scr", name)], dsem=f"d_scr_{name}")
        g["conv"] = True

    held = set()

    def rel_w(*slots):
        for sl_ in slots:
            held.discard(sl_)

    def get_w(name):
        g = wg[name]
        while True:
            slot = ws_ctr[0] % NWS
            ws_ctr[0] += 1
            if slot not in held:
                break
        held.add(slot)
        nk, width = g["nk"], g["width"]
        while not g["conv"]:
            bg_step()
        assert g["conv"], name
        if True:
            P.add("sp", lambda e, slot=slot, nk=nk, width=width, scr=g["scr"]: e.dma_start(out=wslot[slot][:, 0:nk, 0:width], in_=scr),
                  reads=[("scr", name, k0) for k0 in range(0, nk, 2)], writes=[("ws", slot)], dsem=f"d_ws{slot}")
        return slot

    BG_ORDER = (["in1", "in3", "in0", "in2", "in5", "in6", "in4", "out0", "out1"] + [f"up{g}" for g in range(11)]
                + [f"dn{nh}_{jg}" for nh in range(2) for jg in range(3)] + ["pg0", "pp0", "pg1", "pp1"])
    NST = 5
    LOOK = 4

    def conv_gen():
        items = []
        for name in BG_ORDER:
            g = wg[name]
            for k0 in range(0, g["nk"], 2):
                items.append((name, k0, min(g["nk"], k0 + 2)))

        def load(i):
            name, k0, k1 = items[i]
            st = i % NST
            for (dc, w, src) in wg[name]["srcs"]:
                P.add("sp", lambda e, st=st, k0=k0, k1=k1, dc=dc, w=w, src=src: e.dma_start(out=wstage[st][:, 0:k1 - k0, dc:dc + w], in_=src[:, k0:k1, :]),
                      writes=wst_ids(st), dsem=f"d_st{st}")

        for i in range(min(LOOK, len(items))):
            load(i)
        for i, (name, k0, k1) in enumerate(items):
            st = i % NST
            q = i % 4
            width = wg[name]["width"]
            mids = [("mixT", 2 * q), ("mixT", 2 * q + 1)]
            P.add("pool", lambda e, st=st, k0=k0, k1=k1, q=q, width=width: e.tensor_copy(out=mixT[:, 2 * q:2 * q + k1 - k0, 0:width], in_=wstage[st][:, 0:k1 - k0, 0:width]),
                  reads=wst_ids(st), writes=mids)
            P.add("sp", lambda e, k0=k0, k1=k1, q=q, width=width, scr=wg[name]["scr"]: e.dma_start(out=scr[:, k0:k1, :], in_=mixT[:, 2 * q:2 * q + k1 - k0, 0:width]),
                  reads=mids, writes=[("scr", name, k0)], dsem=f"d_sw{q}")
            if i + LOOK < len(items):
                load(i + LOOK)
            if k1 == wg[name]["nk"]:
                wg[name]["conv"] = True
                yield "c"

    def cv(col):
        return cvec[:, col:col + 1]

    def rms_stats(hb, NS):
        h, ss, rs = hT[hb], ssT[hb], rsT[hb]
        for s in range(NS):
            P.add("act", lambda e, s=s: e.activation(out=junk[:], in_=h[:, s, :], func=AF.Square, accum_out=ss[:, s:s + 1]),
                  reads=[("h", hb, s)], writes=["junk", ("ss", hb, s)])
        P.add("dve", lambda e: e.tensor_scalar(out=rs[:, 0:NS], in0=ss[:, 0:NS], scalar1=1.0 / D, scalar2=EPS, op0=ALU.mult, op1=ALU.add),
              reads=[("ss", hb, s) for s in range(NS)], writes=[("rs", hb)])
        P.add("act", lambda e: e.activation(out=rs[:, 0:NS], in_=rs[:, 0:NS], func=AF.Sqrt), reads=[("rs", hb)], writes=[("rs", hb)])
        P.add("dve", lambda e: e.reciprocal(out=rs[:, 0:NS], in_=rs[:, 0:NS]), reads=[("rs", hb)], writes=[("rs", hb)])

    def norm_T(hb, NS, gcol):
        h, rs, yT, ybf = hT[hb], rsT[hb], yTT[hb], ybfT[hb]
        rms_stats(hb, NS)

        def scale(s):
            yb = s % 2
            P.add("act", lambda e, s=s, yb=yb: e.activation(out=ybf[yb][:], in_=h[:, s, :], func=AF.Copy, scale=rs[:, s:s + 1]),
                  reads=[("h", hb, s), ("rs", hb)], writes=[("ybf", hb, yb)])

        scale(0)
        yield "n"
        for s in range(NS):
            yb = s % 2
            b = nb()
            for kc in range(KC):
                P.add("pe", lambda e, b=b, kc=kc, yb=yb: e.transpose(out=bkb[b][:, sl(kc)], in_=ybf[yb][:, sl(kc)], identity=ident[:]),
                      reads=[("ybf", hb, yb), "ident"], writes=[("bk", b)])
            if s + 1 < NS:
                scale(s + 1)
            for kc in range(KC):
                if s % 2 == 0:
                    P.add("dve", lambda e, b=b, kc=kc, s=s: e.tensor_scalar(out=yT[:, kc, sl(s)], in0=bkb[b][:, sl(kc)], scalar1=cv(gcol + kc), scalar2=None, op0=ALU.mult),
                          reads=[("bk", b), "cvec"], writes=[("yT", hb, s)])
                else:
                    P.add("act", lambda e, b=b, kc=kc, s=s: e.activation(out=yT[:, kc, sl(s)], in_=bkb[b][:, sl(kc)], func=AF.Copy, scale=cv(gcol + kc)),
                          reads=[("bk", b), "cvec"], writes=[("yT", hb, s)])
            yield "n"

    def yT_ids(hb, NS):
        return [("yT", hb, s) for s in range(NS)]

    def proj_fm(hb, ws, c0, Tt, NS):
        yT = yTT[hb]
        b = nb()
        for kc in range(KC):
            P.add("pe", lambda e, b=b, kc=kc: e.matmul(bk[b][:, 0:Tt], lhsT=wslot[ws][:, kc, c0:c0 + 128], rhs=yT[:, kc, 0:Tt], start=(kc == 0), stop=(kc == KC - 1)),
                  reads=[("ws", ws)] + yT_ids(hb, NS), writes=[("bk", b)])
        return b

    def resid_add(hb, b, s, nh):
        h = hT[hb]
        P.add("dve", lambda e: e.tensor_tensor(out=h[:, s, sl(nh, 512)], in0=h[:, s, sl(nh, 512)], in1=bk[b][:, 0:512], op=ALU.add),
              reads=[("h", hb, s), ("bk", b)], writes=[("h", hb, s)])

    def tile(mode, src, r0, NS, hb, out_r0=None):
        Tt = NS * 128
        full = mode == "full"
        light = mode == "light"
        h, yT, rs = hT[hb], yTT[hb], rsT[hb]
        P.add("sp", lambda e: e.dma_start(out=h[:, 0:NS, :], in_=src[r0:r0 + Tt, :].rearrange("(s p) d -> p s d", p=128)),
              writes=[("h", hb, s) for s in range(NS)], dsem=f"d_x{hb}")
        yield from norm_T(hb, NS, C_ATTN)
        yield "A"
        ws = get_w("in1")
        for s in range(NS):
            b = nb()
            for kc in range(KC):
                P.add("pe", lambda e, b=b, kc=kc, s=s, ws=ws: e.matmul(bk[b][:, 0:512], lhsT=yT[:, kc, sl(s)], rhs=wslot[ws][:, kc, 0:512], start=(kc == 0), stop=(kc == KC - 1)),
                      reads=[("ws", ws), ("yT", hb, s)], writes=[("bk", b)])
            P.add("act", lambda e, b=b, s=s: e.activation(out=vtok[:, s, :], in_=bk[b][:, 0:512], func=AF.Copy),
                  reads=[("bk", b)], writes=[("vtok", s)])
            yield "c"
        rel_w(ws)
        if not light:
            ws = get_w("in2")
            for c in range(4):
                b = proj_fm(hb, ws, c * 128, Tt, NS)
                P.add("act", lambda e, b=b, c=c: e.activation(out=sg[:, c, 0:Tt], in_=bk[b][:, 0:Tt], func=AF.Silu),
                      reads=[("bk", b)], writes=[("sg", c)])
                yield "c"
        if not light:
            rel_w(ws)
        ws = get_w("in3")
        b = nb()
        for kc in range(KC):
            P.add("pe", lambda e, b=b, kc=kc, ws=ws: e.matmul(bk[b][0:16, 0:Tt], lhsT=wslot[ws][:, kc, 0:16], rhs=yT[:, kc, 0:Tt], start=(kc == 0), stop=(kc == KC - 1)),
                  reads=[("ws", ws)] + yT_ids(hb, NS), writes=[("bk", b)])
        P.add("dve", lambda e, b=b: e.tensor_copy(out=aaug[0:16, 0:Tt], in_=bk[b][0:16, 0:Tt]), reads=[("bk", b)], writes=["aaug"])
        rel_w(ws)
        yield "c"
        for s in range(NS):
            b = nb()
            P.add("pe", lambda e, b=b, s=s: e.matmul(bk[b][:, 0:256], lhsT=aaug[0:17, sl(s)], rhs=walpha[0:17, :], start=True, stop=True),
                  reads=["aaug", "walpha"], writes=[("bk", b)])
            xi = s % 2
            P.add("act", lambda e, b=b, xi=xi: e.activation(out=ex[xi][:], in_=bk[b][:, 0:256], func=AF.Exp, scale=-1.0),
                  reads=[("bk", b)], writes=[("ex", xi)])
            P.add("act", lambda e, s=s, xi=xi: e.activation(out=Lt[:, s, :], in_=ex[xi][:], func=AF.Ln, bias=one1[:, 0:1]),
                  reads=[("ex", xi), "one1"], writes=[("Lt", s)])
            if s % 2 == 1:
                yield "c"
        yield "c"
        for c in range(2):
            b = nb()
            for s in range(NS):
                P.add("pe", lambda e, b=b, s=s, c=c: e.matmul(bk[b][:, sl(s)], lhsT=Lt[:, s, sl(c)], rhs=trineg[:], start=True, stop=True),
                      reads=[("Lt", s), "trineg"], writes=[("bk", b)])
            P.add("act", lambda e, b=b, c=c: e.activation(out=eb[:, c, 0:Tt], in_=bk[b][:, 0:Tt], func=AF.Exp),
                  reads=[("bk", b)], writes=[("eb", c)])
            P.add("act", lambda e, b=b, c=c: e.activation(out=enb[:, c, 0:Tt], in_=bk[b][:, 0:Tt], func=AF.Exp, scale=-1.0),
                  reads=[("bk", b)], writes=[("enb", c)])
            yield "c"
        ws = get_w("in0")
        for c in range(2):
            if not light:
                b = proj_fm(hb, ws, c * 128, Tt, NS)
                for hh in range(2):
                    P.add("dve", lambda e, b=b, c=c, hh=hh: e.scalar_tensor_tensor(out=qdz[hh][:, c, 0:Tt], in0=bk[b][:, 0:Tt], scalar=hm[:, hh:hh + 1], in1=eb[:, c, 0:Tt], op0=ALU.mult, op1=ALU.mult),
                          reads=[("bk", b), ("eb", c), "hm"], writes=[("qd", c, hh)])
                yield "c"
            b = proj_fm(hb, ws, 256 + c * 128, Tt, NS)
            P.add("dve", lambda e, b=b, c=c: e.tensor_tensor(out=kd[:, c, 0:Tt], in0=bk[b][:, 0:Tt], in1=enb[:, c, 0:Tt], op=ALU.mult),
                  reads=[("bk", b), ("enb", c)], writes=[("kd", c)])
            yield "c"
        rel_w(ws)
        for s in range(NS):
            b = nb()
            for c in range(2):
                P.add("pe", lambda e, b=b, c=c, s=s: e.transpose(out=bkb[b][:, sl(c)], in_=kd[:, c, sl(s)], identity=ident[:]),
                      reads=[("kd", c), "ident"], writes=[("bk", b)])
            P.add("dve", lambda e, b=b, s=s: e.tensor_copy(out=kdtok[s][:], in_=bkb[b][:, 0:256]), reads=[("bk", b)], writes=[("kdtok", s)])
            if not light:
                b2 = nb()
                for hh4 in range(4):
                    c, hh = divmod(hh4, 2)
                    P.add("pe", lambda e, b2=b2, hh4=hh4, c=c, hh=hh, s=s: e.matmul(bk[b2][:, sl(hh4)], lhsT=kd[:, c, sl(s)], rhs=qdz[hh][:, c, sl(s)], start=True, stop=True),
                          reads=[("kd", c), ("qd", c, hh)], writes=[("bk", b2)])
                P.add("dve", lambda e, b2=b2, s=s: e.tensor_tensor(out=scm[s][:], in0=bk[b2][:, 0:512], in1=mtri4[:], op=ALU.mult),
                      reads=[("bk", b2), "mtri4"], writes=[("scm", s)])
            yield "c"
        for s in range(NS):
            if not light:
                b3 = nb()
                for hh4 in range(4):
                    c, hh = divmod(hh4, 2)
                    P.add("pe", lambda e, b3=b3, hh4=hh4, s=s: e.matmul(bk[b3][:, sl(hh4)], lhsT=vtok[:, s, sl(hh4)], rhs=scm[s][:, sl(hh4)], start=True, stop=False),
                          reads=[("vtok", s), ("scm", s)], writes=[("bk", b3)])
                    P.add("pe", lambda e, b3=b3, hh4=hh4, c=c, hh=hh, s=s: e.matmul(bk[b3][:, sl(hh4)], lhsT=Sbf[:, c, sl(hh)], rhs=qdz[hh][:, c, sl(s)], start=False, stop=True),
                          reads=[("Sbf", c), ("qd", c, hh)], writes=[("bk", b3)])
                P.add("act", lambda e, b3=b3, s=s: e.activation(out=oT[:, :, sl(s)], in_=bk[b3][:, 0:512].rearrange("p (a i) -> p a i", a=4), func=AF.Copy),
                      reads=[("bk", b3)], writes=[("oT", a) for a in range(4)])
                yield "c"
            for c in range(2):
                b4 = nb()
                P.add("pe", lambda e, b4=b4, c=c, s=s: e.matmul(bk[b4][:, 0:256], lhsT=kdtok[s][:, sl(c)], rhs=vtok[:, s, sl(c, 256)], start=True, stop=True),
                      reads=[("kdtok", s), ("vtok", s)], writes=[("bk", b4)])
                P.add("dve", lambda e, b4=b4, c=c: e.scalar_tensor_tensor(out=R[:, c, :], in0=R[:, c, :], scalar=dsv[:, c:c + 1], in1=bk[b4][:, 0:256], op0=ALU.mult, op1=ALU.add),
                      reads=[("R", c), ("dsv", c), ("bk", b4)], writes=[("R", c)])
                col = s * 128 + 127
                P.add("dve", lambda e, c=c, col=col: e.tensor_copy(out=dsv[:, c:c + 1], in_=eb[:, c, col:col + 1]),
                      reads=[("eb", c)], writes=[("dsv", c)])
                P.add("act", lambda e, c=c, col=col: e.activation(out=Sbf[:, c, :], in_=R[:, c, :], func=AF.Copy, scale=eb[:, c, col:col + 1]),
                      reads=[("R", c), ("eb", c)], writes=[("Sbf", c)])
            yield "c"
        if light:
            yield "M"
            return
        yield "H"
        for a in range(4):
            P.add("act", lambda e, a=a: e.activation(out=osq[:, a, 0:Tt], in_=oT[:, a, 0:Tt], func=AF.Square),
                  reads=[("oT", a)], writes=[("osq", a)])
        yield "c"
        hn = []
        for a in range(4):
            b = nb()
            P.add("pe", lambda e, b=b, a=a: e.matmul(bk[b][:, 0:Tt], lhsT=onesdv[:], rhs=osq[:, a, 0:Tt], start=True, stop=True),
                  reads=["onesdv", ("osq", a)], writes=[("bk", b)])
            t1 = nt()
            P.add("act", lambda e, b=b, t1=t1: e.activation(out=tmp[t1][:, 0:Tt], in_=bk[b][:, 0:Tt], func=AF.Sqrt, bias=epsT[:, 0:1]),
                  reads=[("bk", b), "epsT"], writes=[("tmp", t1)])
            P.add("dve", lambda e, t1=t1: e.reciprocal(out=tmp[t1][:, 0:Tt], in_=tmp[t1][:, 0:Tt]), reads=[("tmp", t1)], writes=[("tmp", t1)])
            P.add("dve", lambda e, t1=t1, a=a: e.tensor_tensor(out=tmp[t1][:, 0:Tt], in0=oT[:, a, 0:Tt], in1=tmp[t1][:, 0:Tt], op=ALU.mult),
                  reads=[("tmp", t1), ("oT", a)], writes=[("tmp", t1)])
            P.add("dve", lambda e, t1=t1, a=a: e.scalar_tensor_tensor(out=mixT[:, a, 0:Tt], in0=tmp[t1][:, 0:Tt], scalar=cv(C_HG + a), in1=sg[:, a, 0:Tt], op0=ALU.mult, op1=ALU.mult),
                  reads=[("tmp", t1), "cvec", ("sg", a)], writes=[("mixT", a)])
            yield "c"
        if not light:
            ws = get_w("in5")
            for c in range(4):
                b = proj_fm(hb, ws, c * 128, Tt, NS)
                P.add("act", lambda e, b=b, c=c: e.activation(out=oT[:, c, 0:Tt], in_=bk[b][:, 0:Tt], func=AF.Copy),
                      reads=[("bk", b)], writes=[("oT", c)])
                yield "c"
            rel_w(ws)
            ws = get_w("in6")
            for c in range(4):
                b = proj_fm(hb, ws, c * 128, Tt, NS)
                P.add("dve", lambda e, b=b, c=c: e.tensor_tensor(out=ubuf[:, c, 2:2 + Tt], in0=bk[b][:, 0:Tt], in1=oT[:, c, 0:Tt], op=ALU.mult),
                      reads=[("bk", b), ("oT", c)], writes=[("ub", c)])
                yield "c"
            rel_w(ws)
            ws = get_w("in4")
            for c in range(4):
                b = proj_fm(hb, ws, c * 128, Tt, NS)
                t1 = nt()
                P.add("act", lambda e, c=c, t1=t1: e.activation(out=tmp[t1][:, 0:Tt], in_=ubuf[:, c, 0:Tt], func=AF.Copy, scale=cv(C_MCW + 0 * 4 + c)),
                      reads=[("ub", c), "ubh", "cvec"], writes=[("tmp", t1)])
                P.add("dve", lambda e, c=c, t1=t1: e.scalar_tensor_tensor(out=tmp[t1][:, 0:Tt], in0=ubuf[:, c, 1:1 + Tt], scalar=cv(C_MCW + 1 * 4 + c), in1=tmp[t1][:, 0:Tt], op0=ALU.mult, op1=ALU.add),
                      reads=[("ub", c), "ubh", "cvec", ("tmp", t1)], writes=[("tmp", t1)])
                P.add("dve", lambda e, c=c, t1=t1: e.scalar_tensor_tensor(out=tmp[t1][:, 0:Tt], in0=ubuf[:, c, 2:2 + Tt], scalar=cv(C_MCW + 2 * 4 + c), in1=tmp[t1][:, 0:Tt], op0=ALU.mult, op1=ALU.add),
                      reads=[("ub", c), "cvec", ("tmp", t1)], writes=[("tmp", t1)])
                P.add("dve", lambda e, c=c, t1=t1, b=b: e.tensor_tensor(out=mixT[:, 4 + c, 0:Tt], in0=tmp[t1][:, 0:Tt], in1=bk[b][:, 0:Tt], op=ALU.mult),
                      reads=[("tmp", t1), ("bk", b)], writes=[("mixT", 4 + c)])
                yield "c"
            rel_w(ws)
            P.add("pool", lambda e: e.tensor_copy(out=ubuf[:, :, 0:2], in_=ubuf[:, :, Tt:Tt + 2]),
                  reads=[("ub", c) for c in range(4)], writes=["ubh"])
        yield "M"
        for nh in range(2):
            ws = get_w(f"out{nh}")
            for s in range(NS):
                b = nb()
                for kc in range(KC):
                    P.add("pe", lambda e, b=b, kc=kc, s=s, ws=ws: e.matmul(bk[b][:, 0:512], lhsT=mixT[:, kc, sl(s)], rhs=wslot[ws][:, kc, 0:512], start=(kc == 0), stop=(kc == KC - 1)),
                          reads=[("ws", ws), ("mixT", kc)], writes=[("bk", b)])
                resid_add(hb, b, s, nh)
                yield "c"
            rel_w(ws)
        yield "W"
        if full:
            P.add("sp", lambda e: e.dma_start(out=pbuf[:, 0:NS, :], in_=p_d[r0:r0 + Tt, :].rearrange("(s p) d -> p s d", p=128)),
                  writes=["pbuf"], dsem="d_p")
            P.add("pool", lambda e: e.tensor_copy(out=pbf[:, 0:NS, :], in_=pbuf[:, 0:NS, :]), reads=["pbuf"], writes=["pbf"])
        yield from norm_T(hb, NS, C_FFN)
        ws = None
        for g in range(11):
            if ws is not None:
                rel_w(ws)
            ws = get_w(f"up{g}")
            for jj in range(2):
                j = 2 * g + jj
                bg = proj_fm(hb, ws, jj * 128, Tt, NS)
                gi = j % 3
                P.add("pool", lambda e, gi=gi, j=j: e.tensor_copy(out=gbuf[gi][:, 0:2], in_=ghalo[:, j, :]),
                      reads=[("gh", j)], writes=[("gbh", gi)])
                P.add("act", lambda e, gi=gi, bg=bg: e.activation(out=gbuf[gi][:, 2:2 + Tt], in_=bk[bg][:, 0:Tt], func=AF.Copy),
                      reads=[("bk", bg)], writes=[("gb", gi)])
                P.add("pool", lambda e, gi=gi, j=j: e.tensor_copy(out=ghalo[:, j, :], in_=gbuf[gi][:, Tt:Tt + 2]),
                      reads=[("gb", gi)], writes=[("gh", j)])
                if not full:
                    yield "c"
                    continue
                bu = proj_fm(hb, ws, 256 + jj * 128, Tt, NS)
                t1 = nt()
                P.add("act", lambda e, gi=gi, j=j, t1=t1: e.activation(out=tmp[t1][:, 0:Tt], in_=gbuf[gi][:, 0:Tt], func=AF.Copy, scale=cv(C_FCW + 0 * NJ + j)),
                      reads=[("gb", gi), ("gbh", gi), "cvec"], writes=[("tmp", t1)])
                P.add("dve", lambda e, gi=gi, j=j, t1=t1: e.scalar_tensor_tensor(out=tmp[t1][:, 0:Tt], in0=gbuf[gi][:, 1:1 + Tt], scalar=cv(C_FCW + 1 * NJ + j), in1=tmp[t1][:, 0:Tt], op0=ALU.mult, op1=ALU.add),
                      reads=[("gb", gi), ("gbh", gi), "cvec", ("tmp", t1)], writes=[("tmp", t1)])
                P.add("dve", lambda e, gi=gi, j=j, t1=t1: e.scalar_tensor_tensor(out=tmp[t1][:, 0:Tt], in0=gbuf[gi][:, 2:2 + Tt], scalar=cv(C_FCW + 2 * NJ + j), in1=tmp[t1][:, 0:Tt], op0=ALU.mult, op1=ALU.add),
                      reads=[("gb", gi), "cvec", ("tmp", t1)], writes=[("tmp", t1)])
                P.add("act", lambda e, t1=t1: e.activation(out=tmp[t1][:, 0:Tt], in_=tmp[t1][:, 0:Tt], func=AF.Silu),
                      reads=[("tmp", t1)], writes=[("tmp", t1)])
                P.add("dve", lambda e, t1=t1, j=j, bu=bu: e.tensor_tensor(out=actT[:, j, 0:Tt], in0=tmp[t1][:, 0:Tt], in1=bk[bu][:, 0:Tt], op=ALU.mult),
                      reads=[("tmp", t1), ("bk", bu)], writes=[("act", j)])
                yield "c"
        rel_w(ws)
        if not full:
            return
        for nh in range(2):
            banks4 = reserve(NS)
            for jg, (j0, j1) in enumerate(DN_GROUPS):
                ws = get_w(f"dn{nh}_{jg}")
                for s in range(NS):
                    for j in range(j0, j1):
                        P.add("pe", lambda e, b=banks4[s], j=j, j0=j0, s=s, ws=ws: e.matmul(bk[b][:, 0:512], lhsT=actT[:, j, sl(s)], rhs=wslot[ws][:, j - j0, 0:512], start=(j == 0), stop=(j == NJ - 1)),
                              reads=[("ws", ws), ("act", j)], writes=[("bk", banks4[s])])
                    yield "c"
                rel_w(ws)
            for s in range(NS):
                resid_add(hb, banks4[s], s, nh)
            release(banks4)
            yield "c"
        yield from norm_T(hb, NS, C_PLE)
        for s in range(NS):
            b = nb()
            for kc in range(2):
                P.add("pe", lambda e, b=b, kc=kc, s=s: e.transpose(out=bkb[b][:, sl(kc)], in_=pbf[:, s, sl(kc)], identity=ident[:]),
                      reads=["pbf", "ident"], writes=[("bk", b)])
            P.add("dve", lambda e, b=b, s=s: e.tensor_copy(out=pT[:, :, sl(s)], in_=bkb[b][:, 0:256].rearrange("p (a i) -> p a i", a=2)),
                  reads=[("bk", b)], writes=[("pT", s)])
        yield "c"
        for nh in range(2):
            wsg = get_w(f"pg{nh}")
            wsp = get_w(f"pp{nh}")
            for s in range(NS):
                bg = nb()
                for kc in range(KC):
                    P.add("pe", lambda e, bg=bg, kc=kc, s=s, wsg=wsg: e.matmul(bk[bg][:, 0:512], lhsT=yT[:, kc, sl(s)], rhs=wslot[wsg][:, kc, 0:512], start=(kc == 0), stop=(kc == KC - 1)),
                          reads=[("ws", wsg), ("yT", hb, s)], writes=[("bk", bg)])
                bp = nb()
                for kc in range(2):
                    P.add("pe", lambda e, bp=bp, kc=kc, s=s, wsp=wsp: e.matmul(bk[bp][:, 0:512], lhsT=pT[:, kc, sl(s)], rhs=wslot[wsp][:, kc, 0:512], start=(kc == 0), stop=(kc == 1)),
                          reads=[("ws", wsp), ("pT", s)], writes=[("bk", bp)])
                t1 = nt()
                P.add("act", lambda e, t1=t1, bg=bg: e.activation(out=tmp[t1][:], in_=bk[bg][:, 0:512], func=AF.Sigmoid),
                      reads=[("bk", bg)], writes=[("tmp", t1)])
                P.add("dve", lambda e, t1=t1, bp=bp: e.tensor_tensor(out=tmp[t1][:], in0=tmp[t1][:], in1=bk[bp][:, 0:512], op=ALU.mult),
                      reads=[("tmp", t1), ("bk", bp)], writes=[("tmp", t1)])
                P.add("pool", lambda e, t1=t1, s=s, nh=nh: e.tensor_tensor(out=h[:, s, sl(nh, 512)], in0=h[:, s, sl(nh, 512)], in1=tmp[t1][:], op=ALU.add),
                      reads=[("tmp", t1), ("h", hb, s)], writes=[("h", hb, s)])
                yield "c"
            rel_w(wsg, wsp)
        rms_stats(hb, NS)
        yield "c"
        for s in range(NS):
            P.add("dve", lambda e, s=s: e.scalar_tensor_tensor(out=h[:, s, :], in0=h[:, s, :], scalar=rs[:, s:s + 1], in1=gfin[:], op0=ALU.mult, op1=ALU.mult),
                  reads=[("h", hb, s), ("rs", hb), "gfin"], writes=[("h", hb, s)])
            P.add("pool", lambda e, s=s: e.dma_start(out=out_d[out_r0 + s * 128:out_r0 + (s + 1) * 128, :], in_=h[:, s, :]),
                  reads=[("h", hb, s)], writes=[("o", out_r0, s)], dsem=f"d_o{hb}")
            yield "c"

    specs = []
    r = 0
    nl = NL
    while nl > 0:
        ns = min(4, nl)
        specs.append(("light", xp_d, r, ns, None))
        r += ns * 128
        nl -= ns
    if HALO:
        specs.append(("halo", xp_d, r, 1, None))
    out_ids = []
    for t in range(NF):
        specs.append(("full", x_d, t * 512, 4, t * 512))
        out_ids += [("o", t * 512, s) for s in range(4)]
    runs = []
    for i, (mode, src, r0, ns, o0) in enumerate(specs):
        runs.append(dict(g=tile(mode, src, r0, ns, i % 2, out_r0=o0), tag=None, fin=False, mode=mode))
    bg = dict(g=conv_gen(), fin=False)

    def bg_step():
        if not bg["fin"]:
            try:
                next(bg["g"])
            except StopIteration:
                bg["fin"] = True

    def one(t):
        try:
            t["tag"] = next(t["g"])
        except StopIteration:
            t["fin"] = True
            t["tag"] = "END"

    def reached(t, targets):
        return t is None or t["fin"] or t["tag"] in targets

    nstep = [0]

    def interleave(a, ta, b, tb, ra=2, rb=1):
        while not (reached(a, ta) and reached(b, tb)):
            for _ in range(ra):
                if reached(a, ta):
                    break
                one(a)
            for _ in range(rb):
                if reached(b, tb):
                    break
                one(b)
            nstep[0] += 1
            if nstep[0] % 2 == 0:
                bg_step()

    def drain_bg():
        while not bg["fin"]:
            bg_step()

    if runs[0]["mode"] != "light":
        drain_bg()
    while not wg["in1"]["conv"]:
        bg_step()
    for i, cur in enumerate(runs):
        nxt = runs[i + 1] if i + 1 < len(runs) else None
        if nxt is not None and nxt["mode"] != "light":
            drain_bg()
        if cur["mode"] == "light":
            interleave(cur, {"W"}, nxt, {"A"})
        else:
            interleave(cur, {"W"}, None, {"A"})
        credit = 0.0
        while not reached(cur, {"END"}):
            one(cur)
            credit += 4.0 if cur["tag"] == "n" else 0.45
            while credit >= 1.0 and not reached(nxt, {"M"}):
                one(nxt)
                credit -= 1.0
            if reached(nxt, {"M"}):
                credit = 0.0
            nstep[0] += 1
            if nstep[0] % 2 == 0:
                bg_step()
    while not bg["fin"]:
        bg_step()
    P.add("sp", None, reads=out_ids)

    stats = P.analyze()
    keys = P.sem_keys()
    sems = {k: es.enter_context(nc.semaphore(f"s{i}")) for i, k in enumerate(keys)}
    with nc.Block() as block:
        P.emit(block, sems)
    es.close()
    return nc, stats


_NAMES = ["attn_norm", "w_in", "w_alpha_up", "b_alpha", "gla_head_norm", "mix_conv_w", "w_out", "ffn_norm",
          "w_up", "ffn_conv_w", "w_down", "ple_norm", "w_ple_gate", "w_ple_proj"]


def make_in_maps(inputs, n_cores=8):
    x = np.asarray(inputs["x"], dtype=np.float32)
    p = np.asarray(inputs["p"], dtype=np.float32)
    common = {k: np.ascontiguousarray(np.asarray(inputs[k], dtype=np.float32)[0]) for k in _NAMES}
    common["final_norm"] = np.ascontiguousarray(np.asarray(inputs["final_norm"], dtype=np.float32))
    maps = []
    for c in range(n_cores):
        b, hf = divmod(c, 2)
        m = dict(common)
        m["x"] = np.ascontiguousarray(x[b, hf * SEQ_HALF:(hf + 1) * SEQ_HALF])
        m["p"] = np.ascontiguousarray(p[0, b, hf * SEQ_HALF:(hf + 1) * SEQ_HALF])
        if hf == 1:
            m["xp"] = np.ascontiguousarray(x[b, 0:SEQ_HALF])
        else:
            m["xp"] = np.zeros((SEQ_HALF, D), np.float32)
        maps.append(m)
    return maps


def kernel(**inputs):
    nc, _ = build_nc()
    maps = make_in_maps(inputs)
    res = run_bass_kernel_spmd(nc, maps, core_ids=list(range(8)))
    x = inputs["x"]
    out = np.empty((4, 2 * SEQ_HALF, D), np.float32)
    for c in range(8):
        b, hf = divmod(c, 2)
        out[b, hf * SEQ_HALF:(hf + 1) * SEQ_HALF] = np.asarray(res.results[c]["out"], dtype=np.float32)
    return out
```
